# Optimizing a Trainium2 kernel written in Bass

```python
import math
import jax, jax.numpy as jnp
from jax import lax
import numpy as np

D_MODEL = 1024
BATCH = 2
SEQ = 8192
DEPTH = 1
DEC_BATCH = 128
DEC_SEQ = 4
PAST_LEN = 2048
PAGE_SIZE = 128

DA_HEAD_DIM = 64
DA_QK = 2 * DA_HEAD_DIM
DA_V_DIM = 2 * DA_HEAD_DIM
DA_HEADS = D_MODEL // DA_V_DIM
DA_WIDTH = DA_HEADS * DA_V_DIM
ATTN_SCALE = DA_HEAD_DIM ** -0.5
Q_BLOCK = 128
NEG_INF = -1e30
RW_HEAD_DIM = 64
RW_HEADS = D_MODEL // RW_HEAD_DIM
RW_WIDTH = RW_HEADS * RW_HEAD_DIM
RW_DECAY_RANK = 64
RW_ICLR_RANK = 64
RW_GATE_RANK = 160
RW_PROJ = 3 * RW_WIDTH + RW_DECAY_RANK + RW_ICLR_RANK + RW_GATE_RANK
GN_EPS = 64e-5
OFF_K = DA_HEADS * DA_QK
OFF_V = 2 * DA_HEADS * DA_QK
OFF_RW = OFF_V + DA_WIDTH
OFF_GATE = OFF_RW + RW_PROJ
IN_WIDTH = OFF_GATE + 2 * D_MODEL
N_BUCKETS = 32
MAX_DISTANCE = 128
N_GROUPS = 4
EXPERTS_PER_GROUP = 8
N_EXPERTS = N_GROUPS * EXPERTS_PER_GROUP
TOP_K_IN_GROUP = 2
EXPERT_HIDDEN = D_MODEL // 4
PLE_DIM = 256
RMS_EPS = 1e-6

kernel_name = 'hybrid_diffattn_rwkv7_hmoe_decode_step'


def _rmsnorm(x, g):
    xf = x.astype(jnp.float32)
    y = xf * lax.rsqrt(jnp.mean(xf * xf, axis=-1, keepdims=True) + RMS_EPS)
    return (y * g.astype(jnp.float32)).astype(x.dtype)


def _t5_bucket(rel):
    n = jnp.maximum(rel, 0)
    max_exact = N_BUCKETS // 2
    nf = jnp.maximum(n, 1).astype(jnp.float32)
    large = max_exact + (jnp.log(nf / max_exact) / math.log(MAX_DISTANCE / max_exact)
                         * (N_BUCKETS - max_exact)).astype(jnp.int32)
    large = jnp.minimum(large, N_BUCKETS - 1)
    return jnp.where(n < max_exact, n, large)


def _diff_attend(q, k, v, q_pos, k_pos, rel_bias, lam):
    s = jnp.einsum('bqhcd,bkhcd->bhcqk', q, k, preferred_element_type=jnp.float32) * ATTN_SCALE
    rel = q_pos[:, None] - k_pos[None, :]
    bias = jnp.transpose(rel_bias.astype(jnp.float32)[_t5_bucket(rel)], (2, 0, 1))
    s = s + bias[None, :, None]
    s = jnp.where((rel >= 0)[None, None, None], s, NEG_INF)
    p = jax.nn.softmax(s, axis=-1)
    pd = p[:, :, 0] - lam * p[:, :, 1]
    return jnp.einsum('bhqk,bkhd->bqhd', pd.astype(v.dtype), v)


def _diff_attend_blocked(q, k, v, rel_bias, lam):
    B, T = q.shape[0], q.shape[1]
    nb = T // Q_BLOCK
    qb = jnp.swapaxes(q.reshape(B, nb, Q_BLOCK, DA_HEADS, 2, DA_HEAD_DIM), 0, 1)
    pos = jnp.arange(T, dtype=jnp.int32)
    qpos = pos.reshape(nb, Q_BLOCK)
    ob = lax.map(lambda a: _diff_attend(a[0], k, v, a[1], pos, rel_bias, lam), (qb, qpos))
    return jnp.swapaxes(ob, 0, 1).reshape(B, T, DA_HEADS, DA_V_DIM)


def _rwkv7(z, prev_row, s0, mu, w0, w2, a0, a2, g2, k_k, k_a, r_k, gn_w, gn_b):
    f32 = jnp.float32
    B, T = z.shape[0], z.shape[1]
    zp = jnp.concatenate([prev_row[:, None, :].astype(z.dtype), z[:, :-1]], axis=1)
    zs = z + (zp - z) * mu
    r = zs[..., :RW_WIDTH]
    k = zs[..., RW_WIDTH:2 * RW_WIDTH]
    v = zs[..., 2 * RW_WIDTH:3 * RW_WIDTH]
    o = 3 * RW_WIDTH
    xw = zs[..., o:o + RW_DECAY_RANK]
    xa = zs[..., o + RW_DECAY_RANK:o + RW_DECAY_RANK + RW_ICLR_RANK]
    xg = zs[..., o + RW_DECAY_RANK + RW_ICLR_RANK:]
    w = -jax.nn.softplus(-(w0 + jnp.tanh(xw) @ w2).astype(f32)) - 0.5
    decay = jnp.exp(-jnp.exp(w))
    a = jax.nn.sigmoid((a0 + xa @ a2).astype(f32))
    g = (jax.nn.sigmoid(xg) @ g2).astype(f32)
    heads = lambda t: t.reshape(B, T, RW_HEADS, RW_HEAD_DIM)
    kk = heads((k * k_k).astype(f32))
    kk = kk * lax.rsqrt(jnp.maximum(jnp.sum(kk * kk, axis=-1, keepdims=True), 1e-24))
    kf = k.astype(f32) * (1.0 + (a - 1.0) * k_a.astype(f32))
    rh, kh, vh, ah, dh = heads(r.astype(f32)), heads(kf), heads(v.astype(f32)), heads(a), heads(decay)

    def step(S, inp):
        r_t, d_t, k_t, v_t, kk_t, a_t = inp
        sa = jnp.einsum('bhij,bhj->bhi', S, -kk_t)
        S = (S * d_t[:, :, None, :] + sa[..., None] * (kk_t * a_t)[:, :, None, :]
             + v_t[..., None] * k_t[:, :, None, :])
        return S, jnp.einsum('bhij,bhj->bhi', S, r_t)

    xs = tuple(jnp.swapaxes(t, 0, 1) for t in (rh, dh, kh, vh, kk, ah))
    S, y = lax.scan(step, s0.astype(f32), xs)
    y = jnp.swapaxes(y, 0, 1)
    mean = jnp.mean(y, axis=-1, keepdims=True)
    var = jnp.mean(jnp.square(y - mean), axis=-1, keepdims=True)
    yn = ((y - mean) * lax.rsqrt(var + GN_EPS)).reshape(B, T, RW_WIDTH) * gn_w.astype(f32) + gn_b.astype(f32)
    bonus = jnp.sum(rh * kh * r_k.astype(f32), axis=-1, keepdims=True) * vh
    out = (yn + bonus.reshape(B, T, RW_WIDTH)) * g
    return out.astype(z.dtype), S.astype(s0.dtype), z[:, -1]


def _hier_moe(h, w_grp, b_grp, w_exp, b_exp, w_gate, w_up, w_down):
    f32 = jnp.float32
    B, T = h.shape[0], h.shape[1]
    pg = jax.nn.softmax((h @ w_grp + b_grp).astype(f32), axis=-1)
    pg_top, gi = lax.top_k(pg, 1)
    le = (h @ w_exp + b_exp).astype(f32).reshape(B, T, N_GROUPS, EXPERTS_PER_GROUP)
    le_g = jnp.take_along_axis(le, gi[..., None], axis=2)[:, :, 0]
    pv, pi = lax.top_k(jax.nn.softmax(le_g, axis=-1), TOP_K_IN_GROUP)
    wsel = pg_top * pv / jnp.sum(pv, axis=-1, keepdims=True)
    eidx = gi * EXPERTS_PER_GROUP + pi
    combine = jnp.sum(jax.nn.one_hot(eidx, N_EXPERTS, dtype=f32) * wsel[..., None], axis=-2)
    gate = jnp.einsum('btd,edf->btef', h, w_gate)
    up = jnp.einsum('btd,edf->btef', h, w_up)
    hid = jax.nn.silu(gate) * up * combine[..., None].astype(h.dtype)
    return jnp.einsum('btef,efd->btd', hid, w_down)


def _layer(x, ple, past_k, past_v, rw_prev, rw_state, lambda_init, rel_bias, lw):
    B, T = x.shape[0], x.shape[1]
    h = _rmsnorm(x, lw['attn_norm'])
    proj = h @ lw['w_in']
    q = _rmsnorm(proj[..., :OFF_K].reshape(B, T, DA_HEADS, 2, DA_HEAD_DIM), lw['q_norm'])
    k = _rmsnorm(proj[..., OFF_K:OFF_V].reshape(B, T, DA_HEADS, 2, DA_HEAD_DIM), lw['k_norm'])
    v = proj[..., OFF_V:OFF_RW].reshape(B, T, DA_HEADS, DA_V_DIM)
    z = proj[..., OFF_RW:OFF_GATE]
    gate_a = proj[..., OFF_GATE:OFF_GATE + D_MODEL]
    gate_b = proj[..., OFF_GATE + D_MODEL:]
    f32 = jnp.float32
    lam = (jnp.exp(jnp.sum(lw['lq1'].astype(f32) * lw['lk1'].astype(f32)))
           - jnp.exp(jnp.sum(lw['lq2'].astype(f32) * lw['lk2'].astype(f32))) + lambda_init)
    if past_k is None:
        o = _diff_attend_blocked(q, k, v, rel_bias, lam)
    else:
        past = past_k.shape[1]
        k_all = jnp.concatenate([past_k.astype(k.dtype), k], axis=1)
        v_all = jnp.concatenate([past_v.astype(v.dtype), v], axis=1)
        q_pos = past + jnp.arange(T, dtype=jnp.int32)
        k_pos = jnp.arange(past + T, dtype=jnp.int32)
        o = _diff_attend(q, k_all, v_all, q_pos, k_pos, rel_bias, lam)
    o_a = (_rmsnorm(o, lw['subln']) * (1.0 - lambda_init)).reshape(B, T, DA_WIDTH)
    o_b, s_new, last_row = _rwkv7(z, rw_prev, rw_state, lw['rw_mu'], lw['rw_w0'], lw['rw_w2'],
                                  lw['rw_a0'], lw['rw_a2'], lw['rw_g2'], lw['rw_k_k'], lw['rw_k_a'],
                                  lw['rw_r_k'], lw['rw_gn_w'], lw['rw_gn_b'])
    merged = jax.nn.sigmoid(gate_a) * o_a + jax.nn.sigmoid(gate_b) * o_b
    x = x + merged @ lw['w_out']
    x = x + _hier_moe(_rmsnorm(x, lw['ffn_norm']), lw['w_grp'], lw['b_grp'], lw['w_exp'], lw['b_exp'],
                      lw['w_gate'], lw['w_up'], lw['w_down'])
    hp = _rmsnorm(x, lw['ple_norm'])
    x = x + jax.nn.sigmoid(hp @ lw['w_ple_gate']) * (ple.astype(x.dtype) @ lw['w_ple_proj'])
    return x, k.reshape(B, T, DA_HEADS, DA_QK), v, s_new, last_row


def setup_inputs(seed: int = 0) -> dict:
    key = jax.random.key(seed)
    ks = iter(jax.random.split(key, 64))
    f32 = jnp.float32

    def nrm(shape, scale):
        return jax.random.normal(next(ks), shape, f32) * scale

    def unif(shape, lo, hi):
        return jax.random.uniform(next(ks), shape, f32, lo, hi)

    n_pages = PAST_LEN // PAGE_SIZE
    n_used = DEC_BATCH * n_pages
    n_pool = (5 * n_used) // 4
    L, D = DEPTH, D_MODEL
    x_prompt = nrm((BATCH, SEQ, D), 1.0)
    x_sample = nrm((DEC_BATCH, DEC_SEQ, D), 1.0)
    p_prompt = nrm((L, BATCH, SEQ, PLE_DIM), 1.0)
    p_sample = nrm((L, DEC_BATCH, DEC_SEQ, PLE_DIM), 1.0)
    cache_k = nrm((L, n_pool, PAGE_SIZE, DA_HEADS, DA_QK), 1.0)
    cache_v = nrm((L, n_pool, PAGE_SIZE, DA_HEADS, DA_V_DIM), 1.0)
    state_wkv = nrm((L, DEC_BATCH, RW_HEADS, RW_HEAD_DIM, RW_HEAD_DIM), 0.3)
    state_shift = nrm((L, DEC_BATCH, RW_PROJ), 1.0)
    page_table = jax.random.permutation(next(ks), n_pool)[:n_used].astype(jnp.int32).reshape(DEC_BATCH, n_pages)
    return {
        'x_prompt': x_prompt, 'x_sample': x_sample, 'p_prompt': p_prompt, 'p_sample': p_sample,
        'cache_k': cache_k, 'cache_v': cache_v, 'state_wkv': state_wkv, 'state_shift': state_shift,
        'page_table': page_table,
        'rel_bias': nrm((N_BUCKETS, DA_HEADS), 0.5),
        'attn_norm': 1.0 + nrm((L, D), 0.02),
        'w_in': nrm((L, D, IN_WIDTH), D ** -0.5),
        'q_norm': 1.0 + nrm((L, DA_HEAD_DIM), 0.02),
        'k_norm': 1.0 + nrm((L, DA_HEAD_DIM), 0.02),
        'lambda_q1': nrm((L, DA_HEAD_DIM), 0.1),
        'lambda_k1': nrm((L, DA_HEAD_DIM), 0.1),
        'lambda_q2': nrm((L, DA_HEAD_DIM), 0.1),
        'lambda_k2': nrm((L, DA_HEAD_DIM), 0.1),
        'subln_norm': 1.0 + nrm((L, DA_V_DIM), 0.02),
        'rw_mu': unif((L, RW_PROJ), 0.0, 1.0),
        'rw_w0': unif((L, RW_WIDTH), -5.0, -1.0),
        'rw_w2': nrm((L, RW_DECAY_RANK, RW_WIDTH), 0.5 * RW_DECAY_RANK ** -0.5),
        'rw_a0': nrm((L, RW_WIDTH), 0.1),
        'rw_a2': nrm((L, RW_ICLR_RANK, RW_WIDTH), RW_ICLR_RANK ** -0.5),
        'rw_g2': nrm((L, RW_GATE_RANK, RW_WIDTH), RW_GATE_RANK ** -0.5),
        'rw_k_k': 0.85 + nrm((L, RW_WIDTH), 0.02),
        'rw_k_a': 1.0 + nrm((L, RW_WIDTH), 0.02),
        'rw_r_k': nrm((L, RW_HEADS, RW_HEAD_DIM), 0.1),
        'rw_gn_w': 1.0 + nrm((L, RW_WIDTH), 0.02),
        'rw_gn_b': nrm((L, RW_WIDTH), 0.02),
        'w_out': nrm((L, D, D), D ** -0.5),
        'ffn_norm': 1.0 + nrm((L, D), 0.02),
        'w_grp': nrm((L, D, N_GROUPS), D ** -0.5),
        'b_grp': nrm((L, N_GROUPS), 0.01),
        'w_exp': nrm((L, D, N_EXPERTS), D ** -0.5),
        'b_exp': nrm((L, N_EXPERTS), 0.01),
        'w_gate': nrm((L, N_EXPERTS, D, EXPERT_HIDDEN), D ** -0.5),
        'w_up': nrm((L, N_EXPERTS, D, EXPERT_HIDDEN), D ** -0.5),
        'w_down': nrm((L, N_EXPERTS, EXPERT_HIDDEN, D), EXPERT_HIDDEN ** -0.5),
        'ple_norm': 1.0 + nrm((L, D), 0.02),
        'w_ple_gate': nrm((L, D, D), D ** -0.5),
        'w_ple_proj': nrm((L, PLE_DIM, D), PLE_DIM ** -0.5),
    }


def reference(x_prompt, x_sample, p_prompt, p_sample, cache_k, cache_v, state_wkv, state_shift,
              page_table, rel_bias, attn_norm, w_in, q_norm, k_norm, lambda_q1, lambda_k1,
              lambda_q2, lambda_k2, subln_norm, rw_mu, rw_w0, rw_w2, rw_a0, rw_a2, rw_g2,
              rw_k_k, rw_k_a, rw_r_k, rw_gn_w, rw_gn_b, w_out, ffn_norm, w_grp, b_grp, w_exp,
              b_exp, w_gate, w_up, w_down, ple_norm, w_ple_gate, w_ple_proj):
    yp, ys = x_prompt, x_sample
    B, DB = x_prompt.shape[0], x_sample.shape[0]
    kp_l, vp_l, sp_l, rp_l, ks_l, vs_l, ss_l, rs_l = [], [], [], [], [], [], [], []
    for l in range(DEPTH):
        lw = {
            'attn_norm': attn_norm[l], 'w_in': w_in[l], 'q_norm': q_norm[l], 'k_norm': k_norm[l],
            'lq1': lambda_q1[l], 'lk1': lambda_k1[l], 'lq2': lambda_q2[l], 'lk2': lambda_k2[l],
            'subln': subln_norm[l], 'rw_mu': rw_mu[l], 'rw_w0': rw_w0[l], 'rw_w2': rw_w2[l],
            'rw_a0': rw_a0[l], 'rw_a2': rw_a2[l], 'rw_g2': rw_g2[l], 'rw_k_k': rw_k_k[l],
            'rw_k_a': rw_k_a[l], 'rw_r_k': rw_r_k[l], 'rw_gn_w': rw_gn_w[l], 'rw_gn_b': rw_gn_b[l],
            'w_out': w_out[l], 'ffn_norm': ffn_norm[l], 'w_grp': w_grp[l], 'b_grp': b_grp[l],
            'w_exp': w_exp[l], 'b_exp': b_exp[l], 'w_gate': w_gate[l], 'w_up': w_up[l],
            'w_down': w_down[l], 'ple_norm': ple_norm[l], 'w_ple_gate': w_ple_gate[l],
            'w_ple_proj': w_ple_proj[l],
        }
        lambda_init = 0.8 - 0.6 * math.exp(-0.3 * l)
        yp, kp, vp, sp, rp = _layer(
            yp, p_prompt[l], None, None, jnp.zeros((B, RW_PROJ), yp.dtype),
            jnp.zeros((B, RW_HEADS, RW_HEAD_DIM, RW_HEAD_DIM), state_wkv.dtype),
            lambda_init, rel_bias, lw)
        past_k = cache_k[l][page_table].reshape(DB, -1, DA_HEADS, 2, DA_HEAD_DIM)
        past_v = cache_v[l][page_table].reshape(DB, -1, DA_HEADS, DA_V_DIM)
        ys, kS, vS, sS, rS = _layer(ys, p_sample[l], past_k, past_v, state_shift[l], state_wkv[l],
                                    lambda_init, rel_bias, lw)
        kp_l.append(kp); vp_l.append(vp); sp_l.append(sp); rp_l.append(rp)
        ks_l.append(kS); vs_l.append(vS); ss_l.append(sS); rs_l.append(rS)
    return (yp, ys, jnp.stack(kp_l), jnp.stack(vp_l), jnp.stack(sp_l), jnp.stack(rp_l),
            jnp.stack(ks_l), jnp.stack(vs_l), jnp.stack(ss_l), jnp.stack(rs_l))
```

```python
import numpy as np
from contextlib import ExitStack
import concourse.bass as bass
import concourse.mybir as mybir
from concourse.bass_utils import run_bass_kernel_spmd

F32, BF16, I32 = mybir.dt.float32, mybir.dt.bfloat16, mybir.dt.int32
ALU = mybir.AluOpType
AF = mybir.ActivationFunctionType
AX = mybir.AxisListType

SAME_ENGINE_SYNC = True


class Buf:
    __slots__ = ("name", "w", "r")

    def __init__(self, name):
        self.name = name
        self.w = None
        self.r = {}


class Sched:
    ENG = ("pe", "act", "dve", "pool", "sp")

    def __init__(self, nc, es, plan=None, dry=False):
        self.nc = nc
        self.es = es
        self.es_main = es
        self.dry = dry
        self.phase = "g"
        self.plan = []
        self.pre = {}
        self.po_stack = ExitStack()
        if plan is not None:
            for (name, shape, dt) in plan:
                self.pre[name] = es.enter_context(nc.sbuf_tensor("sb_" + name, list(shape), dt))
        self.prog = {e: [] for e in self.ENG}
        self.count = {e: 0 for e in self.ENG}
        self.seen = {e: {} for e in self.ENG}
        self.sems = {}
        self.dcount = {}
        self.nsem = 0
        for e in ("pe", "act", "dve", "pool"):
            self._sem("E_" + e)
        self.out_tokens = {}

    def _sem(self, name):
        if name not in self.sems:
            self.sems[name] = self.es_main.enter_context(self.nc.semaphore("s%d" % self.nsem))
            self.nsem += 1
            assert self.nsem <= 100, "too many semaphores"
        return self.sems[name]

    def sb(self, name, shape, dt):
        if self.dry:
            if self.phase == "g":
                self.plan.append((name, tuple(shape), dt))
            return self.nc.dram_tensor("dry_" + name, list(shape), dt).ap()
        if self.phase == "g":
            if name in self.pre:
                return self.pre[name]
            return self.es_main.enter_context(self.nc.sbuf_tensor("sb_" + name, list(shape), dt))
        if self.phase == "po":
            return self.po_stack.enter_context(self.nc.sbuf_tensor("sb_" + name, list(shape), dt))
        return self.es_main.enter_context(self.nc.sbuf_tensor("sb_" + name, list(shape), dt))

    def free_po(self):
        if not self.dry:
            self.po_stack.close()
        self.phase = "s"

    def ps(self, name, shape, dt):
        if self.dry:
            return self.nc.dram_tensor("dryp_" + name, list(shape), dt).ap()
        return self.es_main.enter_context(self.nc.psum_tensor("ps_" + name, list(shape), dt))

    def _waits(self, eng, deps):
        need = {}
        for d in deps:
            if d is None:
                continue
            s, v = d
            if need.get(s, 0) < v:
                need[s] = v
        own = "E_" + eng
        for s, v in need.items():
            if s == own and (not SAME_ENGINE_SYNC or eng == "pe"):
                continue
            if self.seen[eng].get(s, 0) >= v:
                continue
            self.seen[eng][s] = v
            sem = self.sems[s]
            self.prog[eng].append(lambda h, sem=sem, v=v: h.wait_ge(sem, v))

    def _deps(self, reads, writes):
        deps = []
        for b in reads:
            deps.append(b.w)
        for b in writes:
            deps.append(b.w)
            deps.extend(b.r.items())
        return deps

    def _mark(self, tok, reads, writes):
        s, v = tok
        for b in reads:
            if b.r.get(s, 0) < v:
                b.r[s] = v
        for b in writes:
            b.w = tok
            b.r = {}

    def op(self, eng, meth, *args, reads=(), writes=(), **kw):
        self._waits(eng, self._deps(reads, writes))
        self.count[eng] += 1
        n = self.count[eng]
        sem = self.sems["E_" + eng]
        self.prog[eng].append(lambda h, meth=meth, args=args, kw=kw, sem=sem: getattr(h, meth)(*args, **kw).then_inc(sem, 1))
        self._mark(("E_" + eng, n), reads, writes)

    def dma(self, q, out=None, in_=None, stream=None, reads=(), writes=(), is_output=False, meth="dma_start", **kw):
        self._waits(q, self._deps(reads, writes))
        sname = "D_" + stream
        sem = self._sem(sname)
        self.dcount[sname] = self.dcount.get(sname, 0) + 16
        v = self.dcount[sname]
        self.prog[q].append(lambda h, out=out, in_=in_, kw=kw, sem=sem, meth=meth: getattr(h, meth)(out=out, in_=in_, **kw).then_inc(sem, 16))
        self._mark((sname, v), reads, writes)
        if is_output:
            self.out_tokens[sname] = v

    def barrier(self):
        latest = {}
        for e in ("pe", "act", "dve", "pool"):
            if self.count[e]:
                latest["E_" + e] = self.count[e]
        latest.update(self.dcount)
        for eng in self.ENG:
            self._waits(eng, list(latest.items()))

    def finish(self):
        for s, v in self.out_tokens.items():
            if self.seen["sp"].get(s, 0) < v:
                sem = self.sems[s]
                self.prog["sp"].append(lambda h, sem=sem, v=v: h.wait_ge(sem, v))

    def emit(self):
        if self.dry:
            return
        self.finish()
        with self.nc.Block() as block:
            @block.tensor
            def _(h):
                for f in self.prog["pe"]:
                    f(h)

            @block.scalar
            def _(h):
                for f in self.prog["act"]:
                    f(h)

            @block.vector
            def _(h):
                for f in self.prog["dve"]:
                    f(h)

            @block.gpsimd
            def _(h):
                for f in self.prog["pool"]:
                    f(h)

            @block.sync
            def _(h):
                for f in self.prog["sp"]:
                    f(h)

NTB = 17
TOKB = NTB * 128
D = 1024
NE = 32
EH = 256
RMS_EPS = 1e-6


def rmsnorm_T(S, name, src_ap, src_buf, gsc, identb, hT, hT_buf, col0, ptr, ptr_buf, scr, cb=()):
    ss, ss_b, sq, sq_b, hb, hb_b = scr
    S.op("act", "activation", out=sq[:], in_=src_ap, func=AF.Square, accum_out=ss[:, 0:1],
         reads=[src_buf], writes=[sq_b, ss_b])
    S.op("dve", "tensor_scalar", out=ss[:, 1:2], in0=ss[:, 0:1], scalar1=1.0 / D, scalar2=RMS_EPS,
                                          op0=ALU.mult, op1=ALU.add, reads=[ss_b], writes=[ss_b])
    S.op("act", "activation", out=ss[:, 3:4], in_=ss[:, 1:2], func=AF.Sqrt, reads=[ss_b], writes=[ss_b])
    S.op("dve", "reciprocal", out=ss[:, 2:3], in_=ss[:, 3:4], reads=[ss_b], writes=[ss_b])
    S.op("act", "activation", out=hb[:], in_=src_ap, func=AF.Copy, scale=ss[:, 2:3],
         reads=[src_buf, ss_b], writes=[hb_b])
    for c in range(8):
        S.op("pe", "transpose", out=ptr[:, c * 128:(c + 1) * 128], in_=hb[:, c * 128:(c + 1) * 128],
                                             identity=identb[:], reads=[hb_b] + list(cb), writes=[ptr_buf])
    for c in range(8):
        S.op("dve", "tensor_scalar", out=hT[:, c, col0:col0 + 128], in0=ptr[:, c * 128:(c + 1) * 128],
                                                  scalar1=gsc[:, c:c + 1], scalar2=None, op0=ALU.mult,
             reads=[ptr_buf] + list(cb), writes=[hT_buf])


def build_B():
    nc = bass.Bass("TRN2", target_bir_lowering=False)
    dr = lambda n, s, dt=F32, kind="ExternalInput": nc.dram_tensor(n, list(s), dt, kind=kind).ap()
    mT_d = dr("mT", [D, TOKB])
    x_d = dr("x", [TOKB, D])
    p_d = dr("p", [TOKB, 256])
    wout_d = dr("w_out", [D, D])
    gff_d = dr("gff", [128, 8])
    gpl_d = dr("gpl", [128, 8])
    wr_d = dr("wr", [D, 36])
    br_d = dr("br", [1, 36])
    wg_d = dr("w_gate", [NE, D, EH])
    wu_d = dr("w_up", [NE, D, EH])
    wd_d = dr("w_down", [NE, EH, D])
    wpg_d = dr("w_ple_gate", [D, D])
    wpp_d = dr("w_ple_proj", [256, D])
    id_d = dr("ident", [128, 128])
    y_d = dr("y", [TOKB, D], kind="ExternalOutput")

    with ExitStack() as es:
        S = Sched(nc, es)
        identb = S.sb("identb", [128, 128], BF16); identb_b = Buf("identb")
        wout = S.sb("wout", [128, 8, D], BF16); wout_b = Buf("wout")
        wpg = wout; wpg_b = wout_b
        wpp = S.sb("wpp", [128, 2, D], BF16); wpp_b = Buf("wpp")
        wr = S.sb("wr", [128, 8, 36], BF16); wr_b = Buf("wr")
        brt = S.sb("brt", [128, 36], F32); brt_b = Buf("brt")
        gff = S.sb("gff", [128, 8], F32); gff_b = Buf("gff")
        gpl = S.sb("gpl", [128, 8], F32); gpl_b = Buf("gpl")
        acc = S.sb("acc", [128, NTB, D], F32); acc_b = [Buf("acc%d" % i) for i in range(NTB)]
        h2T = S.sb("h2T", [128, 8, TOKB], BF16); h2T_b = [Buf("h2T%d" % i) for i in range(NTB)]
        comb = S.sb("comb", [128, NTB, NE], F32); comb_b = [Buf("comb%d" % i) for i in range(NTB)]
        hidT = S.sb("hidT", [128, 2, TOKB], BF16); hidT_b = [Buf("hidT%d" % i) for i in range(5)]
        mTs = [S.sb("mTs%d" % i, [128, 8, 128], BF16) for i in range(2)]; mTs_b = [Buf("mTs%d" % i) for i in range(2)]
        xts = [S.sb("xts%d" % i, [128, D], F32) for i in range(2)]; xts_b = [Buf("xts%d" % i) for i in range(2)]
        ss = S.sb("ss", [128, 4], F32); ss_b = Buf("ss")
        sq = S.sb("sq", [128, D], BF16); sq_b = Buf("sq")
        hb = S.sb("hb", [128, D], BF16); hb_b = Buf("hb")
        scr = (ss, ss_b, sq, sq_b, hb, hb_b)
        rt = S.sb("rt", [128, 160], F32); rt_b = Buf("rt")
        wge = [S.sb("wge%d" % i, [128, 8, EH], BF16) for i in range(2)]; wge_b = [Buf("wge%d" % i) for i in range(2)]
        wue = [S.sb("wue%d" % i, [128, 8, EH], BF16) for i in range(2)]; wue_b = [Buf("wue%d" % i) for i in range(2)]
        wde = [S.sb("wde%d" % i, [128, 2, D], BF16) for i in range(2)]; wde_b = [Buf("wde%d" % i) for i in range(2)]
        sil = [S.sb("sil%d" % i, [128, 512], F32) for i in range(2)]; sil_b = [Buf("sil%d" % i) for i in range(2)]
        pts = S.sb("pts", [128, 256], BF16); pts_b = Buf("pts")
        pT = S.sb("pT", [128, 2, 128], BF16); pT_b = Buf("pT")
        hpT = S.sb("hpT", [128, 8, 128], BF16); hpT_b = Buf("hpT")
        sg = S.sb("sg", [128, D], F32); sg_b = Buf("sg")
        yt = [S.sb("yt%d" % i, [128, D], F32) for i in range(2)]; yt_b = [Buf("yt%d" % i) for i in range(2)]
        pA = [S.ps("pA%d" % i, [128, 512], F32) for i in range(2)]; pA_b = [Buf("pA%d" % i) for i in range(2)]
        pB = [S.ps("pB%d" % i, [128, 512], F32) for i in range(2)]; pB_b = [Buf("pB%d" % i) for i in range(2)]
        pC = [S.ps("pC%d" % i, [128, 512], F32) for i in range(2)]; pC_b = [Buf("pC%d" % i) for i in range(2)]
        ptr = S.ps("ptr", [128, 1024], BF16); ptr_b = Buf("ptr")
        pR = S.ps("pR", [128, 512], F32); pR_b = Buf("pR")

        S.dma("pool", out=identb[:], in_=id_d, stream="identb", writes=[identb_b])
        for hh in range(2):
            S.dma("pool",
                out=wout[:, :, hh * 512:(hh + 1) * 512],
                in_=wout_d.rearrange("(c p) n -> p c n", p=128)[:, :, hh * 512:(hh + 1) * 512], stream="wout", writes=[wout_b])
        S.dma("pool", out=wr[:], in_=wr_d.rearrange("(c p) n -> p c n", p=128), stream="wr", writes=[wr_b])
        S.dma("sp", out=brt[:], in_=br_d.partition_broadcast(128), stream="brt", writes=[brt_b])
        S.dma("sp", out=gff[:], in_=gff_d, stream="gff", writes=[gff_b])
        S.dma("sp", out=gpl[:], in_=gpl_d, stream="gpl", writes=[gpl_b])

        def load_expert(e):
            s = e % 2
            S.dma("pool", out=wge[s][:], in_=wg_d[e].rearrange("(c p) n -> p c n", p=128), stream="wge%d" % s, writes=[wge_b[s]])
            S.dma("pool", out=wue[s][:], in_=wu_d[e].rearrange("(c p) n -> p c n", p=128), stream="wue%d" % s, writes=[wue_b[s]])
            S.dma("pool", out=wde[s][:], in_=wd_d[e].rearrange("(c p) n -> p c n", p=128), stream="wde%d" % s, writes=[wde_b[s]])

        for i in range(NTB):
            s = i % 2
            r0 = i * 128
            S.dma("pool", out=mTs[s][:], in_=mT_d.rearrange("(c p) t -> p c t", p=128)[:, :, r0:r0 + 128], stream="mTs%d" % s, writes=[mTs_b[s]])
            S.dma("sp", out=xts[s][:], in_=x_d[r0:r0 + 128, :], stream="xts%d" % s, writes=[xts_b[s]])
            for hh in range(2):
                for c in range(8):
                    S.op("pe", "matmul", pA[hh][:], lhsT=mTs[s][:, c, :],
                                                            rhs=wout[:, c, hh * 512:(hh + 1) * 512],
                                                            start=(c == 0), stop=(c == 7),
                         reads=[mTs_b[s], wout_b], writes=[pA_b[hh]])
                S.op("dve", "tensor_tensor", out=acc[:, i, hh * 512:(hh + 1) * 512], in0=pA[hh][:],
                                                           in1=xts[s][:, hh * 512:(hh + 1) * 512], op=ALU.add,
                     reads=[pA_b[hh], xts_b[s]], writes=[acc_b[i]])
            rmsnorm_T(S, "ffn", acc[:, i, :], acc_b[i], gff, identb, h2T, h2T_b[i], r0, ptr, ptr_b, scr, cb=[identb_b, gff_b])
            for c in range(8):
                S.op("pe", "matmul", pR[:, 0:36], lhsT=h2T[:, c, r0:r0 + 128], rhs=wr[:, c, :],
                                                 start=(c == 0), stop=(c == 7),
                     reads=[h2T_b[i], wr_b], writes=[pR_b])
            L = rt[:, 0:36]
            S.op("dve", "tensor_tensor", out=L, in0=pR[:, 0:36], in1=brt[:], op=ALU.add,
                 reads=[pR_b, brt_b], writes=[rt_b])
            lg = rt[:, 0:4]
            le = rt[:, 4:36]
            mg = rt[:, 36:37]; nmg = rt[:, 37:38]; sgs = rt[:, 38:39]; rsg = rt[:, 39:40]
            oh = rt[:, 40:44]; eg = rt[:, 44:48]
            tmp32 = rt[:, 48:80]
            lsel = rt[:, 80:88]; me = rt[:, 88:89]; nme = rt[:, 89:90]; ee = rt[:, 90:98]
            m1 = rt[:, 98:99]; mk1 = rt[:, 99:107]; ee2 = rt[:, 107:115]; m2 = rt[:, 115:116]; mk2 = rt[:, 116:124]
            den = rt[:, 124:125]; wv = rt[:, 125:126]; sel = rt[:, 126:134]
            R = dict(reads=[rt_b], writes=[rt_b])
            S.op("dve", "tensor_reduce", out=mg, in_=lg, axis=AX.X, op=ALU.max, **R)
            S.op("dve", "tensor_scalar", out=nmg, in0=mg, scalar1=-1.0, scalar2=None, op0=ALU.mult, **R)
            S.op("act", "activation", out=eg, in_=lg, func=AF.Exp, bias=nmg, scale=1.0, accum_out=sgs, **R)
            S.op("dve", "reciprocal", out=rsg, in_=sgs, **R)
            S.op("dve", "tensor_scalar", out=oh, in0=lg, scalar1=mg, scalar2=None, op0=ALU.is_equal, **R)
            S.op("dve", "tensor_tensor", out=tmp32.rearrange("p (g e) -> p g e", g=4),
                                                  in0=le.rearrange("p (g e) -> p g e", g=4),
                                                  in1=oh.unsqueeze(2).broadcast_to([128, 4, 8]), op=ALU.mult, **R)
            S.op("dve", "tensor_reduce", out=lsel, in_=tmp32.rearrange("p (g e) -> p e g", g=4),
                                                  axis=AX.X, op=ALU.add, **R)
            S.op("dve", "tensor_reduce", out=me, in_=lsel, axis=AX.X, op=ALU.max, **R)
            S.op("dve", "tensor_scalar", out=nme, in0=me, scalar1=-1.0, scalar2=None, op0=ALU.mult, **R)
            S.op("act", "activation", out=ee, in_=lsel, func=AF.Exp, bias=nme, scale=1.0, **R)
            S.op("dve", "tensor_reduce", out=m1, in_=ee, axis=AX.X, op=ALU.max, **R)
            S.op("dve", "tensor_scalar", out=mk1, in0=ee, scalar1=m1, scalar2=None, op0=ALU.is_equal, **R)
            S.op("dve", "scalar_tensor_tensor", out=ee2, in0=mk1, scalar=-2.0, in1=ee, op0=ALU.mult, op1=ALU.add, **R)
            S.op("dve", "tensor_reduce", out=m2, in_=ee2, axis=AX.X, op=ALU.max, **R)
            S.op("dve", "tensor_scalar", out=mk2, in0=ee2, scalar1=m2, scalar2=None, op0=ALU.is_equal, **R)
            S.op("dve", "tensor_tensor", out=den, in0=m1, in1=m2, op=ALU.add, **R)
            S.op("dve", "reciprocal", out=wv, in_=den, **R)
            S.op("dve", "tensor_tensor", out=wv, in0=wv, in1=rsg, op=ALU.mult, **R)
            S.op("dve", "tensor_tensor", out=mk1, in0=mk1, in1=mk2, op=ALU.add, **R)
            S.op("dve", "scalar_tensor_tensor", out=sel, in0=mk1, scalar=wv, in1=ee, op0=ALU.mult, op1=ALU.mult, **R)
            S.op("dve", "tensor_tensor", out=comb[:, i, :].rearrange("p (g e) -> p g e", g=4),
                                                  in0=oh.unsqueeze(2).broadcast_to([128, 4, 8]),
                                                  in1=sel.unsqueeze(1).broadcast_to([128, 4, 8]), op=ALU.mult,
                 reads=[rt_b], writes=[comb_b[i]])

        load_expert(0)
        tgs = [(0, 512), (512, 512), (1024, 512), (1536, 512), (2048, 128)]
        for e in range(NE):
            s = e % 2
            if e + 1 < NE:
                load_expert(e + 1)
            for gi, (t0, n) in enumerate(tgs):
                tiles = list(range(t0 // 128, (t0 + n) // 128))
                for fc in range(2):
                    pg, pu = pC[fc], pB[fc]
                    for c in range(8):
                        S.op("pe", "matmul", pg[:, 0:n], lhsT=wge[s][:, c, fc * 128:(fc + 1) * 128],
                                                                       rhs=h2T[:, c, t0:t0 + n], start=(c == 0), stop=(c == 7),
                             reads=[wge_b[s]] + [h2T_b[t] for t in tiles], writes=[pC_b[fc]])
                    for c in range(8):
                        S.op("pe", "matmul", pu[:, 0:n], lhsT=wue[s][:, c, fc * 128:(fc + 1) * 128],
                                                                       rhs=h2T[:, c, t0:t0 + n], start=(c == 0), stop=(c == 7),
                             reads=[wue_b[s]] + [h2T_b[t] for t in tiles], writes=[pB_b[fc]])
                    S.op("act", "activation", out=sil[fc][:, 0:n], in_=pg[:, 0:n], func=AF.Silu,
                         reads=[pC_b[fc]], writes=[sil_b[fc]])
                    S.op("dve", "tensor_tensor", out=hidT[:, fc, t0:t0 + n], in0=sil[fc][:, 0:n],
                                                                      in1=pu[:, 0:n], op=ALU.mult,
                         reads=[sil_b[fc], pB_b[fc]], writes=[hidT_b[gi]])
                for i in tiles:
                    r0 = i * 128
                    for hh in range(2):
                        for fc in range(2):
                            S.op("pe", "matmul", pA[hh][:], lhsT=hidT[:, fc, r0:r0 + 128],
                                                                             rhs=wde[s][:, fc, hh * 512:(hh + 1) * 512],
                                                                             start=(fc == 0), stop=(fc == 1),
                                 reads=[hidT_b[gi], wde_b[s]], writes=[pA_b[hh]])
                        S.op("dve", "scalar_tensor_tensor",
                            out=acc[:, i, hh * 512:(hh + 1) * 512], in0=pA[hh][:], scalar=comb[:, i, e:e + 1],
                            in1=acc[:, i, hh * 512:(hh + 1) * 512], op0=ALU.mult, op1=ALU.add,
                            reads=[pA_b[hh], comb_b[i], acc_b[i]], writes=[acc_b[i]])

        for hh in range(2):
            S.dma("pool",
                out=wpg[:, :, hh * 512:(hh + 1) * 512],
                in_=wpg_d.rearrange("(c p) n -> p c n", p=128)[:, :, hh * 512:(hh + 1) * 512], stream="wout", writes=[wpg_b])
        S.dma("pool", out=wpp[:], in_=wpp_d.rearrange("(c p) n -> p c n", p=128), stream="wpp", writes=[wpp_b])
        for i in range(NTB):
            s = i % 2
            r0 = i * 128
            S.dma("pool", out=pts[:], in_=p_d[r0:r0 + 128, :], stream="pts", writes=[pts_b])
            rmsnorm_T(S, "ple", acc[:, i, :], acc_b[i], gpl, identb, hpT, hpT_b, 0, ptr, ptr_b, scr, cb=[identb_b, gpl_b])
            for c in range(2):
                S.op("pe", "transpose", out=ptr[:, c * 128:(c + 1) * 128], in_=pts[:, c * 128:(c + 1) * 128],
                                                     identity=identb[:], reads=[pts_b, identb_b], writes=[ptr_b])
            S.op("dve", "tensor_copy", out=pT[:].rearrange("p c t -> p (c t)"), in_=ptr[:, 0:256],
                 reads=[ptr_b], writes=[pT_b])
            for hh in range(2):
                for c in range(8):
                    S.op("pe", "matmul", pC[hh][:], lhsT=hpT[:, c, :], rhs=wpg[:, c, hh * 512:(hh + 1) * 512],
                                                            start=(c == 0), stop=(c == 7),
                         reads=[hpT_b, wpg_b], writes=[pC_b[hh]])
                for c in range(2):
                    S.op("pe", "matmul", pB[hh][:], lhsT=pT[:, c, :], rhs=wpp[:, c, hh * 512:(hh + 1) * 512],
                                                            start=(c == 0), stop=(c == 1),
                         reads=[pT_b, wpp_b], writes=[pB_b[hh]])
                S.op("act", "activation", out=sg[:, hh * 512:(hh + 1) * 512], in_=pC[hh][:], func=AF.Sigmoid,
                     reads=[pC_b[hh]], writes=[sg_b])
                S.op("dve", "tensor_tensor", out=sg[:, hh * 512:(hh + 1) * 512], in0=sg[:, hh * 512:(hh + 1) * 512],
                                                           in1=pB[hh][:], op=ALU.mult,
                     reads=[sg_b, pB_b[hh]], writes=[sg_b])
            S.op("dve", "tensor_tensor", out=yt[s][:], in0=sg[:], in1=acc[:, i, :], op=ALU.add,
                 reads=[sg_b, acc_b[i]], writes=[yt_b[s]])
            S.dma("sp", out=y_d[r0:r0 + 128, :], in_=yt[s][:], stream="yt%d" % s, reads=[yt_b[s]], is_output=True)
        S.emit()
    return nc
import math

NTOK_A = 16384 + 512
NG_A = NTOK_A // 512
WCOLS = 1312
QK_EPS = 1e-6


def build_A(n_groups=NG_A, do_attn=False, do_rwkv=False, groups=None):
    plan = _build_A(n_groups, do_attn, do_rwkv, groups, None)
    return _build_A(n_groups, do_attn, do_rwkv, groups, plan)


def _build_A(n_groups, do_attn, do_rwkv, groups, plan):
    nc = bass.Bass("TRN2", target_bir_lowering=False)
    dr = lambda n, s, dt=F32, kind="ExternalInput": nc.dram_tensor(n, list(s), dt, kind=kind).ap()
    x_d = dr("x", [NTOK_A, D])
    w_d = dr("w", [D, WCOLS])
    gat_d = dr("gat", [128, 8])
    gqk_d = dr("gqk", [128, 2])
    id_d = dr("ident", [128, 128])
    bo_d = dr("blockones", [128, 128])
    kout_d = dr("kout", [NTOK_A, 128], kind="ExternalOutput")
    vout_d = dr("vout", [NTOK_A, 128], kind="ExternalOutput")
    zs_d = dr("zs", [6, 128, 130], kind="ExternalOutput")

    with ExitStack() as es:
        S = Sched(nc, es, plan=plan, dry=(plan is None))
        identb = S.sb("identb", [128, 128], BF16); identb_b = Buf("identb")
        identf = S.sb("identf", [128, 128], F32); identf_b = Buf("identf")
        bones = S.sb("bones", [128, 128], F32); bones_b = Buf("bones")
        W = S.sb("W", [128, 8, WCOLS], BF16); W_b = Buf("W")
        gat = S.sb("gat", [128, 8], F32); gat_b = Buf("gat")
        gqk = S.sb("gqk", [128, 2], F32); gqk_b = Buf("gqk")
        xts = [S.sb("xts", [128, D], F32)] * 2; xts_b = [Buf("xts")] * 2
        ss = S.sb("ss", [128, 4], F32); ss_b = Buf("ss")
        hb = S.sb("hb", [128, D], BF16); hb_b = Buf("hb")
        sq, sq_b = hb, hb_b
        scr = (ss, ss_b, sq, sq_b, hb, hb_b)
        hT = [S.sb("hT", [128, 8, 512], BF16)] * 2; hT_b = [Buf("hT")] * 2
        ZN = 10
        zt = [S.sb("zt%d" % i, [128, 513], F32) for i in range(ZN)]; zt_b = [Buf("zt%d" % i) for i in range(ZN)]
        sqf = S.sb("sqf", [128, 512], F32); sqf_b = Buf("sqf")
        rstd = S.sb("rstd", [128, 512], F32); rstd_b = Buf("rstd")
        kn = S.sb("kn", [128, 512], F32); kn_b = Buf("kn")
        qn = S.sb("qn", [128, 512], BF16); qn_b = Buf("qn")
        kT_o = [S.sb("kTo", [128, 512], F32)] * 2; kT_o_b = [Buf("kTo")] * 2
        v_o = [S.sb("vo", [128, 512], F32)] * 2; v_o_b = [Buf("vo")] * 2
        zs = S.sb("zs", [128, 6, 130], F32); zs_b = Buf("zs")
        pP = [S.ps("pP%d" % i, [128, 512], F32) for i in range(3)]; pP_b = [Buf("pP%d" % i) for i in range(3)]
        ptr = S.ps("ptr", [128, 1024], BF16); ptr_b = Buf("ptr")
        pS, pS_b = pP[2], pP_b[2]
        pV, pV_b = pP[1], pP_b[1]

        S.op("pool", "memset", zs[:].rearrange("p a b -> p (a b)"), 0.0, writes=[zs_b])
        S.dma("pool", out=identb[:], in_=id_d, stream="identb", writes=[identb_b])
        S.dma("sp", out=identf[:], in_=id_d, stream="identf", writes=[identf_b])
        S.dma("sp", out=bones[:], in_=bo_d, stream="bones", writes=[bones_b])
        S.dma("sp", out=gat[:], in_=gat_d, stream="gat", writes=[gat_b])
        S.dma("sp", out=gqk[:], in_=gqk_d, stream="gqk", writes=[gqk_b])
        for c in range(8):
            S.dma("pool", out=W[:, c, :], in_=w_d[c * 128:(c + 1) * 128, :], stream="W", writes=[W_b])

        rwc_d = dr("rwc", [128, 16])
        w2a2_d = dr("w2a2", [128, 128])
        g2c_d = dr("g2c", [2, 128, 128])
        maskAR_d = dr("maskAR", [128, 256])
        maskSL_d = dr("maskSL", [128, 128])
        scanm_d = dr("scanm", [128, 512])
        wkv_d = dr("wkv", [130, 128, 64], kind="ExternalOutput")
        mT_d = dr("mT", [128, NTOK_A], kind="ExternalOutput")
        cst = lambda name, shape: (S.sb(name, shape, F32), Buf(name))
        rwc, rwc_b = cst("rwc", [128, 16]); w2a2, w2a2_b = cst("w2a2", [128, 128]); g2c, g2c_b = cst("g2c", [128, 2, 128])
        S.phase = "po"
        maskAR, maskAR_b = cst("maskAR", [128, 256]); maskSL, maskSL_b = cst("maskSL", [128, 128]); scanm, scanm_b = cst("scanm", [128, 512])
        S.phase = "g"
        S.dma("sp", out=rwc[:], in_=rwc_d, stream="rwc", writes=[rwc_b])
        S.dma("sp", out=w2a2[:], in_=w2a2_d, stream="w2a2", writes=[w2a2_b])
        S.dma("sp", out=g2c[:], in_=g2c_d.rearrange("k p n -> p k n"), stream="g2c", writes=[g2c_b])
        S.dma("sp", out=maskAR[:], in_=maskAR_d, stream="maskAR", writes=[maskAR_b])
        S.dma("sp", out=maskSL[:], in_=maskSL_d, stream="maskSL", writes=[maskSL_b])
        S.dma("sp", out=scanm[:], in_=scanm_d, stream="scanm", writes=[scanm_b])
        zsh = []; zsh_b = []
        for k in range(6):
            t_, b_ = cst("zsh%d" % k, [128, 512]); zsh.append(t_); zsh_b.append(b_)
        names = ["tmpA", "th", "ld", "av", "sg1", "sg2", "gv", "kk", "kf", "kka", "Lc", "eg"]
        WK = {}
        for nm in names:
            WK[nm] = cst("rk_" + nm, [128, 512])
        WK["dd"] = WK["th"]; WK["yo"] = WK["sg1"]; WK["sgb"] = WK["sg2"]; WK["yT"] = WK["Lc"]
        WK["einv"] = (zsh[3], zsh_b[3]); WK["eprev"] = (zsh[4], zsh_b[4])
        S.phase = "po"
        AR, AR_b = cst("AR", [128, 8, 256])
        BT, BT_b = cst("BT", [128, 8, 128]); KT, KT_b = cst("KT", [128, 8, 128]); VT, VT_b = cst("VT", [128, 8, 128])
        BbT, BbT_b = cst("BbT", [128, 8, 128]); KbT, KbT_b = cst("KbT", [128, 8, 128])
        for t_, b_ in ((AR, AR_b), (BT, BT_b), (KT, KT_b), (VT, VT_b), (BbT, BbT_b), (KbT, KbT_b)):
            S.op("pool", "memset", t_[:].rearrange("p a b -> p (a b)"), 0.0, writes=[b_])
        NR, NR_b = cst("NR", [128, 256]); AK, AK_b = cst("AK", [128, 256])
        Ab = [cst("Ab%d" % i, [128, 128]) for i in range(2)]; Nb = [cst("Nb%d" % i, [128, 128]) for i in range(2)]
        A0, A0_b = cst("A0", [128, 128]); Mi, Mi_b = cst("Mi", [128, 128])
        TR, TR_b = cst("TR", [128, 384]); Xs, Xs_b = cst("Xs", [128, 128]); Us, Us_b = cst("Us", [128, 128])
        Wst, Wst_b = cst("Wst", [128, 128]); wkvo, wkvo_b = cst("wkvo", [128, 64])
        S.phase = "g"
        mTo = [cst("mTo", [128, 512])] * 2
        prr = [0]

        def pnext():
            i = prr[0] % 3; prr[0] += 1
            return pP[i], pP_b[i]

        def mm(out, ob, lhsT, lb, rhs, rb, start=True, stop=True, skip=False):
            if skip:
                S.op("pe", "matmul", out, lhsT=lhsT, rhs=rhs, start=start, stop=stop, skip_group_check=True, reads=[lb, rb], writes=[ob])
            else:
                S.op("pe", "matmul", out, lhsT=lhsT, rhs=rhs, start=start, stop=stop, reads=[lb, rb], writes=[ob])

        def rwkv_group(g, sample=False):
            t0 = g * 512
            tmpA, tmpA_b = WK["tmpA"]
            yT, yT_b = WK["yT"]
            if sample:
                sample_shift()
            for k, zi in enumerate([] if sample else [2, 3, 4, 7, 8, 9]):
                rows = 32 if zi == 9 else 128
                S.op("dve", "tensor_tensor", out=tmpA[0:rows, :], in0=zt[zi][0:rows, 0:512], in1=zt[zi][0:rows, 1:513], op=ALU.subtract,
                     reads=[zt_b[zi]], writes=[tmpA_b])
                S.op("dve", "scalar_tensor_tensor", out=zsh[k][0:rows, :], in0=tmpA[0:rows, :], scalar=rwc[0:rows, k:k + 1],
                     in1=zt[zi][0:rows, 1:513], op0=ALU.mult, op1=ALU.add, reads=[tmpA_b, zt_b[zi], rwc_b], writes=[zsh_b[k]])
            r_, r_b = zsh[0], zsh_b[0]; k_, k_b = zsh[1], zsh_b[1]; v_, v_b = zsh[2], zsh_b[2]
            th, th_b = WK["th"]; ld, ld_b = WK["ld"]; av, av_b = WK["av"]; sg1, sg1_b = WK["sg1"]; sg2, sg2_b = WK["sg2"]
            gv, gv_b = WK["gv"]; kk, kk_b = WK["kk"]; kf, kf_b = WK["kf"]; kka, kka_b = WK["kka"]; Lc, Lc_b = WK["Lc"]
            eg, eg_b = WK["eg"]; einv, einv_b = WK["einv"]; eprev, eprev_b = WK["eprev"]
            S.op("act", "activation", out=th[0:64, :], in_=zsh[3][0:64, :], func=AF.Tanh, reads=[zsh_b[3]], writes=[th_b])
            p, pb = pnext()
            mm(p[:], pb, w2a2[0:64, :], w2a2_b, th[0:64, :], th_b)
            S.op("act", "activation", out=ld[:], in_=p[:], func=AF.Sigmoid, bias=rwc[:, 6:7], scale=1.0, reads=[pb, rwc_b], writes=[ld_b])
            S.op("dve", "tensor_scalar", out=ld[:], in0=ld[:], scalar1=-math.exp(-0.5), scalar2=None, op0=ALU.mult, reads=[ld_b], writes=[ld_b])
            p, pb = pnext()
            mm(p[:], pb, w2a2[64:128, :], w2a2_b, zsh[3][64:128, :], zsh_b[3])
            S.op("act", "activation", out=av[:], in_=p[:], func=AF.Sigmoid, bias=rwc[:, 7:8], scale=1.0, reads=[pb, rwc_b], writes=[av_b])
            S.op("act", "activation", out=sg1[:], in_=zsh[4][:], func=AF.Sigmoid, reads=[zsh_b[4]], writes=[sg1_b])
            S.op("act", "activation", out=sg2[0:32, :], in_=zsh[5][0:32, :], func=AF.Sigmoid, reads=[zsh_b[5]], writes=[sg2_b])
            p, pb = pnext()
            mm(p[:], pb, g2c[:, 0, :], g2c_b, sg1[:], sg1_b, True, False)
            mm(p[:], pb, g2c[0:32, 1, :], g2c_b, sg2[0:32, :], sg2_b, False, True)
            S.op("act", "copy", out=gv[:], in_=p[:], reads=[pb], writes=[gv_b])
            S.op("dve", "tensor_scalar", out=kk[:], in0=k_[:], scalar1=rwc[:, 8:9], scalar2=None, op0=ALU.mult, reads=[k_b, rwc_b], writes=[kk_b])
            S.op("act", "activation", out=sqf[:], in_=kk[:], func=AF.Square, reads=[kk_b], writes=[sqf_b])
            mm(pS[:], pS_b, bones[:], bones_b, sqf[:], sqf_b)
            S.op("dve", "tensor_scalar", out=rstd[:], in0=pS[:], scalar1=1e-24, scalar2=None, op0=ALU.max, reads=[pS_b], writes=[rstd_b])
            S.op("act", "activation", out=rstd[:], in_=rstd[:], func=AF.Sqrt, reads=[rstd_b], writes=[rstd_b])
            S.op("dve", "reciprocal", out=rstd[:], in_=rstd[:], reads=[rstd_b], writes=[rstd_b])
            S.op("dve", "tensor_tensor", out=kk[:], in0=kk[:], in1=rstd[:], op=ALU.mult, reads=[kk_b, rstd_b], writes=[kk_b])
            S.op("dve", "tensor_scalar", out=kf[:], in0=av[:], scalar1=-1.0, scalar2=rwc[:, 9:10], op0=ALU.add, op1=ALU.mult,
                 reads=[av_b, rwc_b], writes=[kf_b])
            S.op("dve", "scalar_tensor_tensor", out=kf[:], in0=kf[:], scalar=1.0, in1=k_[:], op0=ALU.add, op1=ALU.mult,
                 reads=[kf_b, k_b], writes=[kf_b])
            S.op("pool", "tensor_tensor", out=kka[:], in0=kk[:], in1=av[:], op=ALU.mult, reads=[kk_b, av_b], writes=[kka_b])
            if sample:
                sample_scan()
            else:
                S.op("dve", "tensor_tensor_scan", out=Lc[:], data0=scanm[:], data1=ld[:], initial=0.0, op0=ALU.mult, op1=ALU.add,
                     reads=[scanm_b, ld_b], writes=[Lc_b])
                S.op("act", "activation", out=eg[:], in_=Lc[:], func=AF.Exp, reads=[Lc_b], writes=[eg_b])
                S.op("act", "activation", out=einv[:], in_=Lc[:], func=AF.Exp, scale=-1.0, reads=[Lc_b], writes=[einv_b])
                S.op("pool", "tensor_tensor", out=tmpA[:], in0=Lc[:], in1=ld[:], op=ALU.subtract, reads=[Lc_b, ld_b], writes=[tmpA_b])
                S.op("act", "activation", out=eprev[:], in_=tmpA[:], func=AF.Exp, reads=[tmpA_b], writes=[eprev_b])
                c3 = lambda ap: ap.rearrange("p (n t) -> p n t", t=64)
                for hh in range(2):
                    R = slice(hh * 64, (hh + 1) * 64)
                    C = slice(hh * 64, (hh + 1) * 64)
                    C2 = slice(128 + hh * 64, 128 + (hh + 1) * 64)
                    S.op("dve", "scalar_tensor_tensor", out=AR[R, :, C], in0=c3(kk[R, :]), scalar=-1.0, in1=c3(eprev[R, :]),
                         op0=ALU.mult, op1=ALU.mult, reads=[kk_b, eprev_b], writes=[AR_b])
                    S.op("pool", "tensor_tensor", out=AR[R, :, C2], in0=c3(r_[R, :]), in1=c3(eg[R, :]), op=ALU.mult,
                         reads=[r_b, eg_b], writes=[AR_b])
                    S.op("dve", "tensor_tensor", out=BT[R, :, C], in0=c3(kka[R, :]), in1=c3(einv[R, :]), op=ALU.mult,
                         reads=[kka_b, einv_b], writes=[BT_b])
                    S.op("pool", "tensor_tensor", out=KT[R, :, C], in0=c3(kf[R, :]), in1=c3(einv[R, :]), op=ALU.mult,
                         reads=[kf_b, einv_b], writes=[KT_b])
                    S.op("pool", "tensor_copy", out=VT[R, :, C], in_=c3(v_[R, :]), reads=[v_b], writes=[VT_b])
                    gCb = c3(eg[R, :])[:, :, 63:64].broadcast_to([64, 8, 64])
                    S.op("dve", "tensor_tensor", out=BbT[R, :, C], in0=BT[R, :, C], in1=gCb, op=ALU.mult, reads=[BT_b, eg_b], writes=[BbT_b])
                    S.op("pool", "tensor_tensor", out=KbT[R, :, C], in0=KT[R, :, C], in1=gCb, op=ALU.mult, reads=[KT_b, eg_b], writes=[KbT_b])
                if g in (0, 16):
                    S.op("pool", "memset", Wst[:], 0.0, writes=[Wst_b])
                yT, yT_b = WK["yT"]
                for n in range(8):
                    p, pb = pnext()
                    mm(p[:, 0:256], pb, BT[:, n, :], BT_b, AR[:, n, :], AR_b)
                    S.op("dve", "tensor_tensor", out=NR[:], in0=p[:, 0:256], in1=maskAR[:], op=ALU.mult, reads=[pb, maskAR_b], writes=[NR_b])
                    p, pb = pnext()
                    mm(p[:, 0:256], pb, KT[:, n, :], KT_b, AR[:, n, :], AR_b)
                    S.op("dve", "tensor_tensor", out=AK[:], in0=p[:, 0:256], in1=maskAR[:], op=ALU.mult, reads=[pb, maskAR_b], writes=[AK_b])
                    p, pb = pnext()
                    mm(p[:, 0:128], pb, AR[:, n, 0:128], AR_b, BT[:, n, :], BT_b)
                    S.op("dve", "tensor_tensor", out=A0[:], in0=p[:, 0:128], in1=maskSL[:], op=ALU.mult, reads=[pb, maskSL_b], writes=[A0_b])
                    S.op("pool", "tensor_tensor", out=Mi[:], in0=NR[:, 0:128], in1=identf[:], op=ALU.add, reads=[NR_b, identf_b], writes=[Mi_b])
                    Nk, Nk_b, Ak, Ak_b = NR[:, 0:128], NR_b, A0[:], A0_b
                    for lvl in range(1, 6):
                        An, An_b = Ab[lvl % 2]
                        p, pb = pnext()
                        mm(p[:, 0:128], pb, Nk, Nk_b, Ak, Ak_b)
                        S.op("act", "copy", out=An[:], in_=p[:, 0:128], reads=[pb], writes=[An_b])
                        if lvl <= 4:
                            Nn, Nn_b = Nb[lvl % 2]
                            p, pb = pnext()
                            mm(p[:, 0:128], pb, Ak, Ak_b, Nk, Nk_b)
                            S.op("dve", "tensor_copy", out=Nn[:], in_=p[:, 0:128], reads=[pb], writes=[Nn_b])
                        p, pb = pnext()
                        mm(p[:, 0:128], pb, An[:], An_b, Mi[:], Mi_b)
                        S.op("dve", "tensor_tensor", out=Mi[:], in0=p[:, 0:128], in1=Mi[:], op=ALU.add, reads=[pb, Mi_b], writes=[Mi_b])
                        Ak, Ak_b = An[:], An_b
                        if lvl <= 4:
                            Nk, Nk_b = Nn[:], Nn_b
                    p, pb = pnext()
                    for j, (T_, Tb_) in enumerate(((VT, VT_b), (BbT, BbT_b), (KbT, KbT_b))):
                        S.op("pe", "transpose", out=p[:, j * 128:(j + 1) * 128], in_=T_[:, n, :], identity=identf[:],
                             reads=[Tb_, identf_b], writes=[pb])
                    S.op("act", "copy", out=TR[:], in_=p[:, 0:384], reads=[pb], writes=[TR_b])
                    Vb, Bb, Kb = TR[:, 0:128], TR[:, 128:256], TR[:, 256:384]
                    p, pb = pnext()
                    mm(p[:, 0:128], pb, AR[:, n, 0:128], AR_b, Wst[:], Wst_b, True, False)
                    mm(p[:, 0:128], pb, AK[:, 0:128], AK_b, Vb, TR_b, False, True)
                    S.op("act", "copy", out=Xs[:], in_=p[:, 0:128], reads=[pb], writes=[Xs_b])
                    p, pb = pnext()
                    mm(p[:, 0:128], pb, Mi[:], Mi_b, Xs[:], Xs_b)
                    S.op("dve", "tensor_copy", out=Us[:], in_=p[:, 0:128], reads=[pb], writes=[Us_b])
                    p, pb = pnext()
                    mm(p[:, 0:128], pb, Wst[:], Wst_b, AR[:, n, 128:256], AR_b, True, False)
                    mm(p[:, 0:128], pb, Us[:], Us_b, NR[:, 128:256], NR_b, False, False)
                    mm(p[:, 0:128], pb, Vb, TR_b, AK[:, 128:256], AK_b, False, True)
                    S.op("act", "copy", out=yT[0:64, n * 64:(n + 1) * 64], in_=p[0:64, 0:64], reads=[pb], writes=[yT_b])
                    S.op("act", "copy", out=yT[64:128, n * 64:(n + 1) * 64], in_=p[64:128, 64:128], reads=[pb], writes=[yT_b])
                    p, pb = pnext()
                    mm(p[:, 0:128], pb, Bb, TR_b, Us[:], Us_b, True, False)
                    mm(p[:, 0:128], pb, Kb, TR_b, Vb, TR_b, False, True)
                    for hh in range(2):
                        R = slice(hh * 64, (hh + 1) * 64)
                        S.op("dve", "scalar_tensor_tensor", out=Wst[R, :], in0=Wst[R, :], scalar=eg[R, n * 64 + 63:n * 64 + 64], in1=p[R, 0:128],
                             op0=ALU.mult, op1=ALU.add, reads=[Wst_b, eg_b, pb], writes=[Wst_b])
            dd, dd_b = WK["dd"]; yo, yo_b = WK["yo"]; sgb, sgb_b = WK["sgb"]
            mm(pS[:], pS_b, bones[:], bones_b, yT[:], yT_b)
            S.op("dve", "scalar_tensor_tensor", out=dd[:], in0=pS[:], scalar=-1.0 / 64, in1=yT[:], op0=ALU.mult, op1=ALU.add,
                 reads=[pS_b, yT_b], writes=[dd_b])
            S.op("act", "activation", out=sqf[:], in_=dd[:], func=AF.Square, reads=[dd_b], writes=[sqf_b])
            mm(pS[:], pS_b, bones[:], bones_b, sqf[:], sqf_b)
            S.op("dve", "tensor_scalar", out=rstd[:], in0=pS[:], scalar1=1.0 / 64, scalar2=64e-5, op0=ALU.mult, op1=ALU.add,
                 reads=[pS_b], writes=[rstd_b])
            S.op("act", "activation", out=rstd[:], in_=rstd[:], func=AF.Sqrt, reads=[rstd_b], writes=[rstd_b])
            S.op("dve", "reciprocal", out=rstd[:], in_=rstd[:], reads=[rstd_b], writes=[rstd_b])
            S.op("dve", "tensor_tensor", out=dd[:], in0=dd[:], in1=rstd[:], op=ALU.mult, reads=[dd_b, rstd_b], writes=[dd_b])
            S.op("dve", "tensor_scalar", out=yo[:], in0=dd[:], scalar1=rwc[:, 11:12], scalar2=rwc[:, 12:13], op0=ALU.mult, op1=ALU.add,
                 reads=[dd_b, rwc_b], writes=[yo_b])
            S.op("dve", "scalar_tensor_tensor", out=tmpA[:], in0=r_[:], scalar=rwc[:, 10:11], in1=kf[:], op0=ALU.mult, op1=ALU.mult,
                 reads=[r_b, kf_b, rwc_b], writes=[tmpA_b])
            mm(pS[:], pS_b, bones[:], bones_b, tmpA[:], tmpA_b)
            S.op("dve", "tensor_tensor", out=dd[:], in0=pS[:], in1=v_[:], op=ALU.mult, reads=[pS_b, v_b], writes=[dd_b])
            S.op("dve", "tensor_tensor", out=yo[:], in0=yo[:], in1=dd[:], op=ALU.add, reads=[yo_b, dd_b], writes=[yo_b])
            S.op("dve", "tensor_tensor", out=yo[:], in0=yo[:], in1=gv[:], op=ALU.mult, reads=[yo_b, gv_b], writes=[yo_b])
            S.op("act", "activation", out=sgb[:], in_=zt[6][:, 1:513], func=AF.Sigmoid, reads=[zt_b[6]], writes=[sgb_b])
            mo, mo_b = mTo[g % 2]
            S.op("dve", "tensor_tensor", out=mo[:], in0=yo[:], in1=sgb[:], op=ALU.mult, reads=[yo_b, sgb_b], writes=[mo_b])
            if (not sample) and g in (15, 31):
                p, pb = pnext()
                S.op("pe", "transpose", out=p[:, 0:128], in_=Wst[:], identity=identf[:], reads=[Wst_b, identf_b], writes=[pb])
                S.op("act", "copy", out=wkvo[0:64, :], in_=p[0:64, 0:64], reads=[pb], writes=[wkvo_b])
                S.op("act", "copy", out=wkvo[64:128, :], in_=p[64:128, 64:128], reads=[pb], writes=[wkvo_b])
                S.dma("sp", out=wkv_d[0 if g == 15 else 1], in_=wkvo[:], stream="wkvo", reads=[wkvo_b], is_output=True)
        ATT_SCALE = 0.125
        LAMBDA_INIT = 0.2
        rbc_d = dr("rbc", [33, 1])
        b31_d = dr("b31", [1, 1])
        oh_d = dr("bucket_onehot", [33, 383])
        lam_d = dr("lamv", [4, 64])
        sub_d = dr("subg", [128, 1])
        tab_d = dr("tab_scratch", [1, 384], kind="Internal")
        b31c, b31c_b = cst("b31c", [128, 1]); DE, DE_b = cst("DE", [128, 256]); subg, subg_b = cst("subg", [128, 1])
        S.phase = "po"
        rbc, rbc_b = cst("rbc", [33, 1]); oh, oh_b = cst("oh", [33, 383]); tabr, tabr_b = cst("tabr", [1, 384])
        S.phase = "g"
        lamt, lamt_b = cst("lamt", [128, 4, 64]); lamw, lamw_b = cst("lamw", [128, 8])
        tab_b = Buf("tab_dram")
        S.dma("sp", out=rbc[:], in_=rbc_d, stream="rbc", writes=[rbc_b])
        S.dma("sp", out=b31c[:], in_=b31_d.partition_broadcast(128), stream="b31c", writes=[b31c_b])
        S.dma("sp", out=oh[:], in_=oh_d, stream="oh", writes=[oh_b])
        S.dma("sp", out=subg[:], in_=sub_d, stream="subg", writes=[subg_b])
        S.dma("sp", out=lamt[:].rearrange("p a b -> p (a b)"), in_=lam_d.rearrange("a b -> (a b)").partition_broadcast(128),
              stream="lamt", writes=[lamt_b])
        S.op("dve", "tensor_scalar", out=rbc[0:32, :], in0=rbc[0:32, :], scalar1=b31c[0:32, 0:1], scalar2=8.0, op0=ALU.subtract, op1=ALU.mult,
             reads=[rbc_b, b31c_b], writes=[rbc_b])
        S.op("dve", "memset", rbc[32:33, :], -240000.0, writes=[rbc_b])
        p, pb = pnext()
        mm(p[0:1, 0:383], pb, rbc[:, :], rbc_b, oh[:, :], oh_b)
        S.op("dve", "tensor_copy", out=tabr[:, 0:383], in_=p[0:1, 0:383], reads=[pb], writes=[tabr_b])
        S.dma("sp", out=tab_d[:, 0:383], in_=tabr[:, 0:383], stream="tabw", reads=[tabr_b], writes=[tab_b])
        for kq in range(128):
            S.dma("sp", out=DE[kq:kq + 1, :], in_=tab_d[:, 127 - kq:127 - kq + 256], stream="DE", reads=[tab_b], writes=[DE_b])
        DEh = S.sb("DEh", [128, 256], BF16); DEl = S.sb("DEl", [128, 256], BF16); DEh_b = Buf("DEh")
        S.phase = "po"
        DEt, DEt_b = cst("DEt", [128, 256])
        S.phase = "g"
        S.op("dve", "tensor_copy", out=DEh[:], in_=DE[:], reads=[DE_b], writes=[DEh_b])
        S.op("dve", "tensor_copy", out=DEt[:], in_=DEh[:], reads=[DEh_b], writes=[DEt_b])
        S.op("dve", "tensor_tensor", out=DEt[:], in0=DE[:], in1=DEt[:], op=ALU.subtract, reads=[DE_b, DEt_b], writes=[DEt_b])
        S.op("dve", "tensor_copy", out=DEl[:], in_=DEt[:], reads=[DEt_b], writes=[DEh_b])
        S.op("dve", "tensor_scalar", out=subg[:], in0=subg[:], scalar1=1.0 - LAMBDA_INIT, scalar2=None, op0=ALU.mult, reads=[subg_b], writes=[subg_b])
        S.op("dve", "tensor_tensor", out=lamt[:, 0, :], in0=lamt[:, 0, :], in1=lamt[:, 1, :], op=ALU.mult, reads=[lamt_b], writes=[lamt_b])
        S.op("dve", "tensor_tensor", out=lamt[:, 2, :], in0=lamt[:, 2, :], in1=lamt[:, 3, :], op=ALU.mult, reads=[lamt_b], writes=[lamt_b])
        S.op("dve", "tensor_reduce", out=lamw[:, 0:1], in_=lamt[:, 0, :], axis=AX.X, op=ALU.add, reads=[lamt_b], writes=[lamw_b])
        S.op("dve", "tensor_reduce", out=lamw[:, 1:2], in_=lamt[:, 2, :], axis=AX.X, op=ALU.add, reads=[lamt_b], writes=[lamw_b])
        S.op("act", "activation", out=lamw[:, 2:4], in_=lamw[:, 0:2], func=AF.Exp, reads=[lamw_b], writes=[lamw_b])
        S.op("dve", "tensor_tensor", out=lamw[:, 4:5], in0=lamw[:, 3:4], in1=lamw[:, 2:3], op=ALU.subtract, reads=[lamw_b], writes=[lamw_b])
        S.op("dve", "tensor_scalar", out=lamw[:, 4:5], in0=lamw[:, 4:5], scalar1=-LAMBDA_INIT, scalar2=None, op0=ALU.add, reads=[lamw_b], writes=[lamw_b])

        kTb = S.sb("kTb", [128, 8192], BF16); kTb_b = [Buf("kTb%d" % i) for i in range(16)]
        Vaug = S.sb("Vaug", [128, 64, 130], BF16); Vaug_b = [Buf("Vaug%d" % i) for i in range(16)]
        vaug1_b = Buf("vaug_ones")
        S.op("pool", "memset", Vaug[:, :, 128:130], 1.0, writes=[vaug1_b] + Vaug_b)
        S.phase = "po"
        Qblk = S.sb("Qblk", [128, 2, 512], BF16); qTb_b = Buf("Qblk")
        S.op("pool", "memset", Qblk[:].rearrange("p a b -> p (a b)"), 0.0, writes=[qTb_b])
        PT = [S.sb("PT%d" % i, [128, 2, 256], BF16) for i in range(2)]; PT_b = [Buf("PT%d" % i) for i in range(2)]
        S.phase = "g"
        oaT, oaT_b = cst("oaT", [128, 512])
        S.phase = "po"
        ob_, ob_b = cst("o_blk", [128, 128]); osq, osq_b = DEt, DEt_b; fin, fin_b = cst("fin", [128, 8])
        S.phase = "g"
        pQK = [S.ps("pQK%d" % i, [128, 512], F32) for i in range(2)]; pQK_b = [Buf("pQK%d" % i) for i in range(2)]
        pAC = [S.ps("pAC%d" % i, [128, 512], F32) for i in range(2)]; pAC_b = [Buf("pAC%d" % i) for i in range(2)]
        acc_loc = {(0, 0): (0, 0), (0, 1): (0, 130), (1, 0): (0, 260), (1, 1): (1, 0)}
        jj = [0]

        def attn_group(g, gs):
            gi = g % 16
            S.op("pool", "tensor_copy", out=kTb[:, gi * 512:(gi + 1) * 512], in_=kn[:], reads=[kn_b], writes=[kTb_b[gi]])
            S.op("pool", "tensor_copy", out=Qblk[0:64, :, 0:256], in_=qn[0:64, :].rearrange("p (h q) -> p h q", h=2), reads=[qn_b], writes=[qTb_b])
            S.op("pool", "tensor_copy", out=Qblk[64:128, :, 256:512], in_=qn[64:128, :].rearrange("p (h q) -> p h q", h=2), reads=[qn_b], writes=[qTb_b])
            S.op("pool", "tensor_copy", out=Vaug[:, 4 * gi:4 * gi + 4, 0:128], in_=v_o[gs][:].rearrange("p (t d) -> p t d", d=128),
                 reads=[v_o_b[gs]], writes=[Vaug_b[gi]])
            for hf in range(2):
                i0 = 4 * gi + 2 * hf
                for j in range(i0 + 2):
                    lo = max(0, j - i0)
                    c0 = lo * 128
                    s = jj[0] % 2; jj[0] += 1
                    kb = kTb_b[j // 4]
                    nb = 0
                    biases = []
                    if j == i0 - 1:
                        biases = [(0, 128, 128, 256)]
                    elif j == i0:
                        biases = [(0, 256, 0, 256)]
                    elif j == i0 + 1:
                        biases = [(128, 256, 0, 128)]
                    mm(pQK[s][:, :], pQK_b[s], kTb[:, j * 128:(j + 1) * 128], kb, Qblk[:, hf, :], qTb_b, True, not biases, skip=True)
                    for c in range(2):
                        for (a0_, a1_, d0, d1) in biases:
                            mm(pQK[s][:, c * 256 + a0_:c * 256 + a1_], pQK_b[s], identb[:], identb_b, DEh[:, d0:d1], DEh_b, False, False, skip=True)
                            mm(pQK[s][:, c * 256 + a0_:c * 256 + a1_], pQK_b[s], identb[:], identb_b, DEl[:, d0:d1], DEh_b, False, True, skip=True)
                    S.op("act", "activation", out=PT[s][:, :, c0:256], in_=pQK[s][:].rearrange("p (c q) -> p c q", c=2)[:, :, c0:256],
                         func=AF.Exp, bias=b31c[:, 0:1], scale=ATT_SCALE, reads=[pQK_b[s], b31c_b], writes=[PT_b[s]])
                    for c in range(2):
                        for ll in range(lo, 2):
                            bk, col = acc_loc[(c, ll)]
                            mm(pAC[bk][:, col:col + 130], pAC_b[bk], PT[s][:, c, ll * 128:(ll + 1) * 128], PT_b[s],
                               Vaug[:, j, :], Vaug_b[j // 4], (j == 0 and (c, ll) in ((0, 0), (1, 1))), j == i0 + ll, skip=True)
                for ll in range(2):
                    b0, c0_ = acc_loc[(0, ll)]; b1, c1_ = acc_loc[(1, ll)]
                    F = dict(reads=[fin_b], writes=[fin_b])
                    S.op("dve", "reciprocal", out=fin[:, 0:1], in_=pAC[b0][:, c0_ + 128:c0_ + 129], reads=[pAC_b[b0]], writes=[fin_b])
                    S.op("dve", "reciprocal", out=fin[:, 1:2], in_=pAC[b1][:, c1_ + 128:c1_ + 129], reads=[pAC_b[b1]], writes=[fin_b])
                    S.op("dve", "tensor_tensor", out=fin[:, 1:2], in0=fin[:, 1:2], in1=lamw[:, 4:5], op=ALU.mult, reads=[fin_b, lamw_b], writes=[fin_b])
                    S.op("dve", "tensor_scalar", out=ob_[:], in0=pAC[b0][:, c0_:c0_ + 128], scalar1=fin[:, 0:1], scalar2=None, op0=ALU.mult,
                         reads=[pAC_b[b0], fin_b], writes=[ob_b])
                    S.op("dve", "scalar_tensor_tensor", out=ob_[:], in0=pAC[b1][:, c1_:c1_ + 128], scalar=fin[:, 1:2], in1=ob_[:],
                         op0=ALU.mult, op1=ALU.add, reads=[pAC_b[b1], fin_b, ob_b], writes=[ob_b])
                    S.op("act", "activation", out=osq[:, 0:128], in_=ob_[:], func=AF.Square, accum_out=fin[:, 2:3], reads=[ob_b], writes=[osq_b, fin_b])
                    S.op("dve", "tensor_scalar", out=fin[:, 3:4], in0=fin[:, 2:3], scalar1=1.0 / 128, scalar2=RMS_EPS, op0=ALU.mult, op1=ALU.add, **F)
                    S.op("act", "activation", out=fin[:, 4:5], in_=fin[:, 3:4], func=AF.Sqrt, **F)
                    S.op("dve", "reciprocal", out=fin[:, 5:6], in_=fin[:, 4:5], **F)
                    S.op("dve", "tensor_scalar", out=ob_[:], in0=ob_[:], scalar1=fin[:, 5:6], scalar2=None, op0=ALU.mult,
                         reads=[ob_b, fin_b], writes=[ob_b])
                    p, pb = pnext()
                    S.op("pe", "transpose", out=p[:, 0:128], in_=ob_[:], identity=identf[:], reads=[ob_b, identf_b], writes=[pb])
                    q0 = (2 * hf + ll) * 128
                    S.op("dve", "tensor_scalar", out=oaT[:, q0:q0 + 128], in0=p[:, 0:128], scalar1=subg[:, 0:1], scalar2=None, op0=ALU.mult,
                         reads=[pb, subg_b], writes=[oaT_b])

        kc_d = dr("kc", [2560 * 128, 128])
        vc_d = dr("vc", [2560 * 128, 128])
        pt_d = dr("pt", [1, 2048], I32)
        wkv0_d = dr("wkv0", [128, 128, 64])
        sst_d = dr("sst", [6, 128, 128])
        rowv_d = dr("rowv_scratch", [5, 512, 128], kind="Internal")
        rowv_b = Buf("rowv_dram")
        SMP = {}

        def sample_setup():
            S.barrier()
            S.free_po()
            SMP["idx"] = S.sb("s_idx", [128, 512], I32), Buf("s_idx")
            SMP["ptb"] = S.sb("s_ptb", [128, 512], I32), Buf("s_ptb")
            SMP["idxf"] = cst("s_idxf", [128, 512])
            SMP["io"] = cst("s_io", [128, 1]); SMP["ioi"] = S.sb("s_ioi", [128, 1], I32), Buf("s_ioi")
            SMP["Qs"] = S.sb("s_Qs", [128, 128, 8], BF16), Buf("s_Qs")
            SMP["BN"] = cst("s_BN", [128, 32, 8]); SMP["BNt"] = cst("s_BNt", [128, 32, 8])
            SMP["BNh"] = S.sb("s_BNh", [128, 32, 8], BF16), Buf("s_BNh"); SMP["BNl"] = S.sb("s_BNl", [128, 32, 8], BF16), Buf("s_BNl")
            SMP["DEs"] = S.sb("s_DEs", [128, 2, 8], BF16), Buf("s_DEs")
            SMP["PTs"] = [(S.sb("s_PTs%d" % i, [128, 136], BF16), Buf("s_PTs%d" % i)) for i in range(2)]
            SMP["OA"] = cst("s_OA", [4, 8, 260]); SMP["o1"] = cst("s_o1", [4, 8, 128]); SMP["o2"] = cst("s_o2", [4, 8, 128])
            SMP["fs"] = cst("s_fs", [4, 64])
            SMP["Bc"] = [cst("s_Bc%d" % i, [128, 8, 64]) for i in range(5)]
            SMP["Sst"] = cst("s_Sst", [128, 8, 64]); SMP["tS"] = cst("s_tS", [128, 8, 64]); SMP["sa"] = cst("s_sa", [128, 8])
            SMP["RV"] = cst("s_RV", [128, 512]); SMP["dec"] = cst("s_dec", [128, 512])
            SMP["sstt"] = cst("s_sstt", [128, 128]); SMP["zp"] = cst("s_zp", [128, 512])
            io, io_b = SMP["io"]; ioi, ioi_b = SMP["ioi"]
            S.op("pool", "iota", ioi[:], pattern=[[0, 1]], base=0, channel_multiplier=1, writes=[ioi_b])
            S.op("dve", "tensor_copy", out=io[:], in_=ioi[:], reads=[ioi_b], writes=[io_b])
            BN, BN_b = SMP["BN"]; BNt, BNt_b = SMP["BNt"]; BNh, BNh_b = SMP["BNh"]; BNl, BNl_b = SMP["BNl"]
            S.op("pool", "memset", BN[:].rearrange("p a b -> p (a b)"), -240000.0, writes=[BN_b])
            for sl in range(32):
                for c in range(2):
                    S.dma("sp", out=BN[sl * 4:(sl + 1) * 4, sl, c * 4:(c + 1) * 4], in_=DE[0:4, 0:4], stream="BN", reads=[DE_b], writes=[BN_b])
            fl = lambda ap: ap.rearrange("p a b -> p (a b)")
            S.op("dve", "tensor_copy", out=fl(BNh[:]), in_=fl(BN[:]), reads=[BN_b], writes=[BNh_b])
            S.op("dve", "tensor_copy", out=fl(BNt[:]), in_=fl(BNh[:]), reads=[BNh_b], writes=[BNt_b])
            S.op("dve", "tensor_tensor", out=fl(BNt[:]), in0=fl(BN[:]), in1=fl(BNt[:]), op=ALU.subtract, reads=[BN_b, BNt_b], writes=[BNt_b])
            S.op("dve", "tensor_copy", out=fl(BNl[:]), in_=fl(BNt[:]), reads=[BNt_b], writes=[BNl_b])
            DEs, DEs_b = SMP["DEs"]
            for c in range(2):
                S.op("dve", "tensor_copy", out=DEs[:, 0, c * 4:(c + 1) * 4], in_=DEh[:, 128:132], reads=[DEh_b], writes=[DEs_b])
                S.op("dve", "tensor_copy", out=DEs[:, 1, c * 4:(c + 1) * 4], in_=DEl[:, 128:132], reads=[DEh_b], writes=[DEs_b])

        def sample_attn(gs):
            idx, idx_b = SMP["idx"]; ptb, ptb_b = SMP["ptb"]; idxf, idxf_b = SMP["idxf"]; io, io_b = SMP["io"]
            Qs, Qs_b = SMP["Qs"]; BNh, BNh_b = SMP["BNh"]; BNl, BNl_b = SMP["BNl"]; DEs, DEs_b = SMP["DEs"]
            OA, OA_b = SMP["OA"]; o1, o1_b = SMP["o1"]; o2, o2_b = SMP["o2"]; fs, fs_b = SMP["fs"]
            allk = kTb_b + Vaug_b
            kp_b = [Buf("kp0"), Buf("kp1")]; vp_b = [Buf("vp0"), Buf("vp1")]; knb_b = Buf("knb"); vn_b = Buf("vnew")
            S.op("pool", "memset", Qs[:].rearrange("p a b -> p (a b)"), 0.0, writes=[Qs_b])
            S.op("pool", "tensor_copy", out=Qs[0:64, :, 0:4], in_=qn[0:64, :].rearrange("p (s t) -> p s t", t=4), reads=[qn_b], writes=[Qs_b])
            S.op("pool", "tensor_copy", out=Qs[64:128, :, 4:8], in_=qn[64:128, :].rearrange("p (s t) -> p s t", t=4), reads=[qn_b], writes=[Qs_b])
            S.op("pool", "tensor_copy", out=kTb[:, 4096:4608], in_=kn[:], reads=[kn_b], writes=[knb_b] + allk)
            S.op("pool", "tensor_copy", out=Vaug[:, 32:36, 0:128], in_=v_o[gs][:].rearrange("p (t d) -> p t d", d=128),
                 reads=[v_o_b[gs]], writes=[vn_b])
            for s in range(128):
                if s % 32 == 0:
                    c0 = s * 16
                    S.dma("sp", out=ptb[:], in_=pt_d[:, c0:c0 + 512].partition_broadcast(128), stream="s_ptb", writes=[ptb_b])
                    S.op("dve", "tensor_copy", out=idxf[:], in_=ptb[:], reads=[ptb_b], writes=[idxf_b])
                    S.op("dve", "tensor_scalar", out=idxf[:], in0=idxf[:], scalar1=128.0, scalar2=io[:, 0:1], op0=ALU.mult, op1=ALU.add,
                         reads=[idxf_b, io_b], writes=[idxf_b])
                    S.op("dve", "tensor_copy", out=idx[:], in_=idxf[:], reads=[idxf_b], writes=[idx_b])
                sl = s % 2
                ti = s // 32
                kp = kTb[:, sl * 2048:(sl + 1) * 2048].rearrange("p (g t) -> p g t", t=128)
                for pg in range(16):
                    ic = (s % 32) * 16 + pg
                    S.dma("pool", out=kp[:, pg, :], in_=kc_d, stream="kp%d" % sl, reads=[idx_b], writes=[kp_b[sl]], meth="indirect_dma_start",
                          out_offset=None, in_offset=bass.IndirectOffsetOnAxis(ap=idx[:, ic:ic + 1], axis=0))
                    S.dma("pool", out=Vaug[:, sl * 16 + pg, 0:128], in_=vc_d, stream="vp%d" % sl, reads=[idx_b], writes=[vp_b[sl]],
                          meth="indirect_dma_start", out_offset=None, in_offset=bass.IndirectOffsetOnAxis(ap=idx[:, ic:ic + 1], axis=0))
                pss, pss_b = pQK[s % 2], pQK_b[s % 2]
                for pg in range(16):
                    mm(pss[:, pg * 8:(pg + 1) * 8], pss_b, kp[:, pg, :], kp_b[sl], Qs[:, s, :], Qs_b, True, pg != 15, skip=True)
                mm(pss[:, 120:128], pss_b, identb[:], identb_b, DEs[:, 0, :], DEs_b, False, False, skip=True)
                mm(pss[:, 120:128], pss_b, identb[:], identb_b, DEs[:, 1, :], DEs_b, False, True, skip=True)
                mm(pss[:, 128:136], pss_b, kTb[:, 4096 + ti * 128:4096 + (ti + 1) * 128], knb_b, Qs[:, s, :], Qs_b, True, False, skip=True)
                mm(pss[:, 128:136], pss_b, identb[:], identb_b, BNh[:, s % 32, :], BNh_b, False, False, skip=True)
                mm(pss[:, 128:136], pss_b, identb[:], identb_b, BNl[:, s % 32, :], BNl_b, False, True, skip=True)
                PTs, PTs_b = SMP["PTs"][s % 2]
                S.op("act", "activation", out=PTs[:], in_=pss[:, 0:136], func=AF.Exp, bias=b31c[:, 0:1], scale=ATT_SCALE,
                     reads=[pss_b, b31c_b], writes=[PTs_b])
                pa, pa_b = pAC[s % 2], pAC_b[s % 2]
                for c in range(2):
                    for pg in range(17):
                        rhs, rb = (Vaug[:, sl * 16 + pg, :], vp_b[sl]) if pg < 16 else (Vaug[:, 32 + ti, :], vn_b)
                        mm(pa[0:4, c * 130:(c + 1) * 130], pa_b, PTs[:, pg * 8 + c * 4:pg * 8 + c * 4 + 4], PTs_b, rhs, rb,
                           (c == 0 and pg == 0), pg == 16, skip=True)
                k8 = s % 8
                S.op("act", "copy", out=OA[0:4, k8, :], in_=pa[0:4, 0:260], reads=[pa_b], writes=[OA_b])
                if k8 == 7:
                    b8 = s // 8
                    bc = lambda ap: ap.broadcast_to([4, 8, 128])
                    S.op("dve", "reciprocal", out=fs[:, 0:8], in_=OA[:, :, 128], reads=[OA_b], writes=[fs_b])
                    S.op("dve", "reciprocal", out=fs[:, 8:16], in_=OA[:, :, 258], reads=[OA_b], writes=[fs_b])
                    S.op("dve", "tensor_scalar", out=fs[:, 8:16], in0=fs[:, 8:16], scalar1=lamw[0:4, 4:5], scalar2=None, op0=ALU.mult,
                         reads=[fs_b, lamw_b], writes=[fs_b])
                    S.op("dve", "tensor_tensor", out=o1[:], in0=OA[:, :, 0:128], in1=bc(fs[:, 0:8].unsqueeze(2)), op=ALU.mult,
                         reads=[OA_b, fs_b], writes=[o1_b])
                    S.op("dve", "tensor_tensor", out=o2[:], in0=OA[:, :, 130:258], in1=bc(fs[:, 8:16].unsqueeze(2)), op=ALU.mult,
                         reads=[OA_b, fs_b], writes=[o2_b])
                    S.op("dve", "tensor_tensor", out=o1[:], in0=o1[:], in1=o2[:], op=ALU.add, reads=[o1_b, o2_b], writes=[o1_b])
                    S.op("dve", "tensor_tensor", out=o2[:], in0=o1[:], in1=o1[:], op=ALU.mult, reads=[o1_b], writes=[o2_b])
                    S.op("dve", "tensor_reduce", out=fs[:, 16:24], in_=o2[:], axis=AX.X, op=ALU.add, reads=[o2_b], writes=[fs_b])
                    S.op("dve", "tensor_scalar", out=fs[:, 24:32], in0=fs[:, 16:24], scalar1=1.0 / 128, scalar2=RMS_EPS, op0=ALU.mult, op1=ALU.add,
                         reads=[fs_b], writes=[fs_b])
                    S.op("act", "activation", out=fs[:, 32:40], in_=fs[:, 24:32], func=AF.Sqrt, reads=[fs_b], writes=[fs_b])
                    S.op("dve", "reciprocal", out=fs[:, 40:48], in_=fs[:, 32:40], reads=[fs_b], writes=[fs_b])
                    S.op("dve", "tensor_tensor", out=o1[:], in0=o1[:], in1=bc(fs[:, 40:48].unsqueeze(2)), op=ALU.mult,
                         reads=[o1_b, fs_b], writes=[o1_b])
                    p, pb = pnext()
                    for k in range(8):
                        S.op("pe", "transpose", out=p[:, k * 4:(k + 1) * 4], in_=o1[0:4, k, :], identity=identf[0:4, 0:4],
                             reads=[o1_b, identf_b], writes=[pb])
                    S.op("dve", "tensor_scalar", out=oaT[:, b8 * 32:(b8 + 1) * 32], in0=p[:, 0:32], scalar1=subg[:, 0:1], scalar2=None, op0=ALU.mult,
                         reads=[pb, subg_b], writes=[oaT_b])

        def sample_shift():
            sstt, sstt_b = SMP["sstt"]; zp, zp_b = SMP["zp"]
            tmpA, tmpA_b = WK["tmpA"]
            z4 = lambda ap: ap.rearrange("p (s t) -> p s t", t=4)
            for k, zi in enumerate([2, 3, 4, 7, 8, 9]):
                rows = 32 if zi == 9 else 128
                S.dma("sp", out=sstt[0:rows, :], in_=sst_d[k, 0:rows, :], stream="s_sstt", writes=[sstt_b])
                S.op("pool", "tensor_copy", out=z4(zp[0:rows, :])[:, :, 0], in_=sstt[0:rows, :], reads=[sstt_b], writes=[zp_b])
                S.op("pool", "tensor_copy", out=z4(zp[0:rows, :])[:, :, 1:4], in_=z4(zt[zi][0:rows, 1:513])[:, :, 0:3], reads=[zt_b[zi]], writes=[zp_b])
                S.op("dve", "tensor_tensor", out=tmpA[0:rows, :], in0=zp[0:rows, :], in1=zt[zi][0:rows, 1:513], op=ALU.subtract,
                     reads=[zp_b, zt_b[zi]], writes=[tmpA_b])
                S.op("dve", "scalar_tensor_tensor", out=zsh[k][0:rows, :], in0=tmpA[0:rows, :], scalar=rwc[0:rows, k:k + 1],
                     in1=zt[zi][0:rows, 1:513], op0=ALU.mult, op1=ALU.add, reads=[tmpA_b, zt_b[zi], rwc_b], writes=[zsh_b[k]])

        def sample_scan():
            RV, RV_b = SMP["RV"]; dec, dec_b = SMP["dec"]; Sst, Sst_b = SMP["Sst"]; tS, tS_b = SMP["tS"]; sa, sa_b = SMP["sa"]
            Bc = SMP["Bc"]
            ld, ld_b = WK["ld"]; kk, kk_b = WK["kk"]; kka, kka_b = WK["kka"]; kf, kf_b = WK["kf"]
            yT, yT_b = WK["yT"]
            S.op("act", "activation", out=dec[:], in_=ld[:], func=AF.Exp, reads=[ld_b], writes=[dec_b])
            vecs = [(kk, kk_b), (kka, kka_b), (dec, dec_b), (kf, kf_b), (zsh[0], zsh_b[0])]
            for vi, (vt, vb) in enumerate(vecs):
                p, pb = pnext()
                for ti in range(4):
                    S.op("pe", "transpose", out=p[:, ti * 128:(ti + 1) * 128], in_=vt[:, ti * 128:(ti + 1) * 128], identity=identf[:],
                         reads=[vb, identf_b], writes=[pb])
                S.op("act", "copy", out=RV[:], in_=p[:], reads=[pb], writes=[RV_b])
                S.dma("sp", out=rowv_d[vi].rearrange("(t p) d -> p t d", p=128), in_=RV[:].rearrange("p (t d) -> p t d", d=128),
                      stream="s_RV", reads=[RV_b], writes=[rowv_b])
            v4 = zsh[2][:].rearrange("p (s t) -> p s t", t=4)
            y4 = yT[:].rearrange("p (s t) -> p s t", t=4)
            for b8 in range(16):
                s0 = b8 * 8
                S.dma("sp", out=Sst[:], in_=wkv0_d[s0:s0 + 8].rearrange("s p j -> p s j"), stream="s_Sst", writes=[Sst_b])
                for t in range(4):
                    for vi in range(5):
                        bt, bb = Bc[vi]
                        for hh in range(2):
                            src = rowv_d[vi, s0 * 4 + t:(s0 + 7) * 4 + t + 1:4, hh * 64:(hh + 1) * 64].partition_broadcast(64)
                            S.dma("sp", out=bt[hh * 64:(hh + 1) * 64, :, :], in_=src, stream="s_Bc%d" % vi, reads=[rowv_b], writes=[bb])
                    (KKb, KKb_b), (KKAb, KKAb_b), (Db, Db_b), (Kb_, Kb_b), (Rb, Rb_b) = Bc
                    bc = lambda ap: ap.unsqueeze(2).broadcast_to([128, 8, 64])
                    S.op("dve", "tensor_tensor", out=tS[:], in0=Sst[:], in1=KKb[:], op=ALU.mult, reads=[Sst_b, KKb_b], writes=[tS_b])
                    S.op("dve", "tensor_reduce", out=sa[:], in_=tS[:], axis=AX.X, op=ALU.add, reads=[tS_b], writes=[sa_b])
                    S.op("dve", "tensor_tensor", out=Sst[:], in0=Sst[:], in1=Db[:], op=ALU.mult, reads=[Sst_b, Db_b], writes=[Sst_b])
                    S.op("pool", "tensor_tensor", out=tS[:], in0=KKAb[:], in1=bc(sa[:]), op=ALU.mult, reads=[KKAb_b, sa_b], writes=[tS_b])
                    S.op("dve", "tensor_tensor", out=Sst[:], in0=Sst[:], in1=tS[:], op=ALU.subtract, reads=[Sst_b, tS_b], writes=[Sst_b])
                    S.op("pool", "tensor_tensor", out=tS[:], in0=Kb_[:], in1=bc(v4[:, s0:s0 + 8, t]), op=ALU.mult, reads=[Kb_b, zsh_b[2]], writes=[tS_b])
                    S.op("dve", "tensor_tensor", out=Sst[:], in0=Sst[:], in1=tS[:], op=ALU.add, reads=[Sst_b, tS_b], writes=[Sst_b])
                    S.op("pool", "tensor_tensor", out=tS[:], in0=Sst[:], in1=Rb[:], op=ALU.mult, reads=[Sst_b, Rb_b], writes=[tS_b])
                    S.op("dve", "tensor_reduce", out=y4[:, s0:s0 + 8, t], in_=tS[:], axis=AX.X, op=ALU.add, reads=[tS_b], writes=[yT_b])
                S.dma("sp", out=wkv_d[2 + s0:2 + s0 + 8].rearrange("s p j -> p s j"), in_=Sst[:], stream="s_Sout", reads=[Sst_b], is_output=True)

        mtiles = [(0, 128, 0), (128, 128, 1), (384, 128, 2), (512, 128, 3), (640, 128, 4), (768, 128, 5), (896, 128, 6),
                  (1024, 128, 7), (1152, 128, 8), (1280, 32, 9)]
        pcnt = 0
        if groups is None:
            groups = list(range(n_groups))
        for g in groups:
            if g == 32 and (do_attn or do_rwkv):
                sample_setup()
            gs = g % 2
            t0 = g * 512
            for ti in range(4):
                s = (g * 4 + ti) % 2
                r0 = t0 + ti * 128
                S.dma("sp", out=xts[s][:], in_=x_d[r0:r0 + 128, :], stream="xts%d" % s, writes=[xts_b[s]])
                rmsnorm_T(S, "attn", xts[s][:], xts_b[s], gat, identb, hT[gs], hT_b[gs], ti * 128, ptr, ptr_b, scr, cb=[identb_b, gat_b])
            for (c0, rows, zi) in mtiles:
                pp = pcnt % 3; pcnt += 1
                for c in range(8):
                    S.op("pe", "matmul", pP[pp][0:rows, :], lhsT=W[:, c, c0:c0 + rows], rhs=hT[gs][:, c, :],
                         start=(c == 0), stop=(c == 7), reads=[W_b, hT_b[gs]], writes=[pP_b[pp]])
                if zi in (2, 3, 4, 7, 8, 9):
                    if g in (0, 16, 32):
                        S.op("pool", "memset", zt[zi][0:rows, 0:1], 0.0, writes=[zt_b[zi]])
                    else:
                        S.op("pool", "tensor_copy", out=zt[zi][0:rows, 0:1], in_=zt[zi][0:rows, 512:513], reads=[zt_b[zi]], writes=[zt_b[zi]])
                S.op("act", "copy", out=zt[zi][0:rows, 1:513], in_=pP[pp][0:rows, :], reads=[pP_b[pp]], writes=[zt_b[zi]])
            for ti in range(4):
                for c in range(8):
                    S.op("pe", "matmul", pV[:, ti * 128:(ti + 1) * 128], lhsT=hT[gs][:, c, ti * 128:(ti + 1) * 128],
                         rhs=W[:, c, 256:384], start=(c == 0), stop=(c == 7), reads=[W_b, hT_b[gs]], writes=[pV_b])
            S.op("dve", "tensor_copy", out=v_o[gs][:], in_=pV[:], reads=[pV_b], writes=[v_o_b[gs]])
            S.dma("sp", out=vout_d[t0:t0 + 512, :].rearrange("(t p) d -> p t d", p=128),
                  in_=v_o[gs][:].rearrange("p (t d) -> p t d", d=128), stream="vo%d" % gs, reads=[v_o_b[gs]], is_output=True)
            for qi in range(2):
                S.op("act", "activation", out=sqf[:], in_=zt[qi][:, 1:513], func=AF.Square, reads=[zt_b[qi]], writes=[sqf_b])
                S.op("pe", "matmul", pS[:], lhsT=bones[:], rhs=sqf[:], start=True, stop=True,
                     reads=[bones_b, sqf_b], writes=[pS_b])
                S.op("dve", "tensor_scalar", out=rstd[:], in0=pS[:], scalar1=1.0 / 64, scalar2=QK_EPS, op0=ALU.mult, op1=ALU.add,
                     reads=[pS_b], writes=[rstd_b])
                S.op("act", "activation", out=rstd[:], in_=rstd[:], func=AF.Sqrt, reads=[rstd_b], writes=[rstd_b])
                S.op("dve", "reciprocal", out=rstd[:], in_=rstd[:], reads=[rstd_b], writes=[rstd_b])
                dst, dst_b = (qn, qn_b) if qi == 0 else (kn, kn_b)
                S.op("dve", "scalar_tensor_tensor", out=dst[:], in0=zt[qi][:, 1:513], scalar=gqk[:, qi:qi + 1], in1=rstd[:],
                     op0=ALU.mult, op1=ALU.mult, reads=[zt_b[qi], rstd_b, gqk_b], writes=[dst_b])
            for ti in range(4):
                S.op("pe", "transpose", out=pS[:, ti * 128:(ti + 1) * 128], in_=kn[:, ti * 128:(ti + 1) * 128], identity=identf[:],
                     reads=[kn_b, identf_b], writes=[pS_b])
            S.op("act", "copy", out=kT_o[gs][:], in_=pS[:], reads=[pS_b], writes=[kT_o_b[gs]])
            S.dma("sp", out=kout_d[t0:t0 + 512, :].rearrange("(t p) d -> p t d", p=128),
                  in_=kT_o[gs][:].rearrange("p (t d) -> p t d", d=128), stream="kTo%d" % gs, reads=[kT_o_b[gs]], is_output=True)
            if do_attn:
                if g < 32:
                    attn_group(g, gs)
                else:
                    sample_attn(gs)
            if do_rwkv:
                rwkv_group(g, sample=(g == 32))
                mo, mo_b = mTo[g % 2]
                if do_attn:
                    sga, sga_b = WK["sgb"]
                    S.op("act", "activation", out=sga[:], in_=zt[5][:, 1:513], func=AF.Sigmoid, reads=[zt_b[5]], writes=[sga_b])
                    S.op("dve", "tensor_tensor", out=sga[:], in0=sga[:], in1=oaT[:], op=ALU.mult, reads=[sga_b, oaT_b], writes=[sga_b])
                    S.op("dve", "tensor_tensor", out=mo[:], in0=mo[:], in1=sga[:], op=ALU.add, reads=[mo_b, sga_b], writes=[mo_b])
                S.dma("sp", out=mT_d[:, t0:t0 + 512], in_=mo[:], stream="mTo%d" % (g % 2), reads=[mo_b], is_output=True)
            zrows = [2, 3, 4, 7, 8, 9]
            if g in (15, 31):
                col = 0 if g == 15 else 1
                for k, zi in enumerate(zrows):
                    S.op("dve", "tensor_copy", out=zs[0:(32 if zi == 9 else 128), k, col:col + 1], in_=zt[zi][0:(32 if zi == 9 else 128), 512:513], reads=[zt_b[zi]], writes=[zs_b])
            if g == 32:
                for k, zi in enumerate(zrows):
                    S.op("dve", "tensor_copy", out=zs[0:(32 if zi == 9 else 128), k, 2:130], in_=zt[zi][0:(32 if zi == 9 else 128), 1:513].rearrange("p (s t) -> p s t", t=4)[:, :, 3],
                         reads=[zt_b[zi]], writes=[zs_b])
        S.dma("sp", out=zs_d.rearrange("k p n -> p k n"), in_=zs[:], stream="zs", reads=[zs_b], is_output=True)
        S.emit()
        if plan is None:
            return S.plan
    return nc

OFF_K, OFF_V, OFF_RW, OFF_GATE = 1024, 2048, 3072, 6432
_NC_CACHE = {}


def _core_cols(c):
    cols = []
    cols += list(range(c * 128, (c + 1) * 128))
    cols += list(range(OFF_K + c * 128, OFF_K + (c + 1) * 128))
    cols += list(range(OFF_V + c * 128, OFF_V + (c + 1) * 128))
    for j in range(3):
        cols += list(range(OFF_RW + j * 1024 + c * 128, OFF_RW + j * 1024 + (c + 1) * 128))
    cols += list(range(OFF_GATE + c * 128, OFF_GATE + (c + 1) * 128))
    cols += list(range(OFF_GATE + 1024 + c * 128, OFF_GATE + 1024 + (c + 1) * 128))
    cols += list(range(OFF_RW + 3072, OFF_RW + 3360))
    return np.array(cols)


def _consts():
    f32 = np.float32
    bo = np.kron(np.eye(2), np.ones((64, 64))).astype(f32)
    su = np.triu(np.ones((64, 64)), 1); iu = np.triu(np.ones((64, 64)), 0)
    bd = lambda m: np.kron(np.eye(2), m).astype(f32)
    scanm = np.ones((128, 512), f32); scanm[:, ::64] = 0
    rel = np.arange(-127, 256)
    n = np.maximum(rel, 0); nf = np.maximum(n, 1).astype(f32)
    large = 16 + (np.log(nf / 16) / math.log(128 / 16) * 16).astype(np.int32); large = np.minimum(large, 31)
    bucket = np.where(n < 16, n, large); bucket = np.where(rel < 0, 32, bucket)
    oh = np.zeros((33, 383), f32); oh[bucket, np.arange(383)] = 1
    return dict(ident=np.eye(128, dtype=f32), blockones=bo, maskAR=np.concatenate([bd(su), bd(iu)], 1), maskSL=bd(su.T),
                scanm=scanm, bucket_onehot=oh)


def kernel(x_prompt, x_sample, p_prompt, p_sample, cache_k, cache_v, state_wkv, state_shift,
           page_table, rel_bias, attn_norm, w_in, q_norm, k_norm, lambda_q1, lambda_k1,
           lambda_q2, lambda_k2, subln_norm, rw_mu, rw_w0, rw_w2, rw_a0, rw_a2, rw_g2,
           rw_k_k, rw_k_a, rw_r_k, rw_gn_w, rw_gn_b, w_out, ffn_norm, w_grp, b_grp, w_exp,
           b_exp, w_gate, w_up, w_down, ple_norm, w_ple_gate, w_ple_proj):
    f32 = np.float32
    A = lambda a: np.ascontiguousarray(np.asarray(a, dtype=f32))
    xp = A(x_prompt).reshape(16384, 1024)
    xs = A(x_sample).reshape(512, 1024)
    x_all = np.concatenate([xp, xs], 0)
    w_in0 = A(w_in)[0]
    ident = np.eye(128, dtype=f32)
    gat = A(A(attn_norm)[0].reshape(8, 128).T)
    gqk = A(np.stack([np.tile(A(q_norm)[0], 2), np.tile(A(k_norm)[0], 2)], 1))
    if "A" not in _NC_CACHE:
        _NC_CACHE["A"] = build_A(do_attn=True, do_rwkv=True)
    C = _consts()
    mu = A(rw_mu)[0]; ck = A(cache_k)[0]; cv = A(cache_v)[0]; sw = A(state_wkv)[0]; ssh = A(state_shift)[0]
    lamv = A(np.stack([A(lambda_q1)[0], A(lambda_k1)[0], A(lambda_q2)[0], A(lambda_k2)[0]], 0))
    ptab = np.ascontiguousarray(np.asarray(page_table, dtype=np.int32).reshape(1, 2048))
    in_a = []
    for c in range(8):
        cs = slice(c * 128, (c + 1) * 128)
        rwc = np.zeros((128, 16), f32)
        rwc[:, 0] = mu[c * 128:(c + 1) * 128]; rwc[:, 1] = mu[1024 + c * 128:1024 + (c + 1) * 128]; rwc[:, 2] = mu[2048 + c * 128:2048 + (c + 1) * 128]
        rwc[:, 3] = mu[3072:3200]; rwc[:, 4] = mu[3200:3328]; rwc[:32, 5] = mu[3328:3360]
        for i, vv in enumerate([rw_w0, rw_a0, rw_k_k, rw_k_a, rw_r_k, rw_gn_w, rw_gn_b]):
            rwc[:, 6 + i] = A(vv)[0].reshape(-1)[cs]
        g2 = A(rw_g2)[0][:, cs]
        g2c = np.zeros((2, 128, 128), f32); g2c[0] = g2[:128]; g2c[1, :32] = g2[128:]
        rb = A(rel_bias)[:, c]
        sst = np.zeros((6, 128, 128), f32)
        sst[0] = ssh[:, c * 128:(c + 1) * 128].T; sst[1] = ssh[:, 1024 + c * 128:1024 + (c + 1) * 128].T
        sst[2] = ssh[:, 2048 + c * 128:2048 + (c + 1) * 128].T
        sst[3] = ssh[:, 3072:3200].T; sst[4] = ssh[:, 3200:3328].T; sst[5, :32] = ssh[:, 3328:3360].T
        in_a.append(dict(
            x=x_all, w=A(w_in0[:, _core_cols(c)]), gat=gat, gqk=gqk, rwc=rwc,
            w2a2=A(np.concatenate([A(rw_w2)[0][:, cs], A(rw_a2)[0][:, cs]], 0)), g2c=g2c,
            rbc=A(np.concatenate([rb, [0.0]])[:, None]), b31=A(rb[31:32][None]), lamv=lamv, subg=A(A(subln_norm)[0][:, None]),
            kc=np.ascontiguousarray(ck[:, :, c, :].transpose(0, 2, 1)).reshape(2560 * 128, 128),
            vc=np.ascontiguousarray(cv[:, :, c, :]).reshape(2560 * 128, 128),
            pt=ptab, wkv0=np.ascontiguousarray(sw[:, 2 * c:2 * c + 2]).reshape(128, 128, 64), sst=sst, **C))
    ra = run_bass_kernel_spmd(_NC_CACHE["A"], in_a, core_ids=list(range(8))).results
    k_prompt = np.zeros((1, 2, 8192, 8, 128), f32); v_prompt = np.zeros((1, 2, 8192, 8, 128), f32)
    k_sample = np.zeros((1, 128, 4, 8, 128), f32); v_sample = np.zeros((1, 128, 4, 8, 128), f32)
    shift_prompt = np.zeros((1, 2, 3360), f32); shift_sample = np.zeros((1, 128, 3360), f32)
    wkv_prompt = np.zeros((1, 2, 16, 64, 64), f32); wkv_sample = np.zeros((1, 128, 16, 64, 64), f32)
    mT = np.zeros((1024, 16896), f32)
    for c in range(8):
        r = ra[c]
        k_prompt[0, :, :, c, :] = r["kout"][:16384].reshape(2, 8192, 128)
        v_prompt[0, :, :, c, :] = r["vout"][:16384].reshape(2, 8192, 128)
        k_sample[0, :, :, c, :] = r["kout"][16384:].reshape(128, 4, 128)
        v_sample[0, :, :, c, :] = r["vout"][16384:].reshape(128, 4, 128)
        zs = r["zs"]
        for j in range(3):
            shift_prompt[0, :, j * 1024 + c * 128:j * 1024 + (c + 1) * 128] = zs[j][:, 0:2].T
            shift_sample[0, :, j * 1024 + c * 128:j * 1024 + (c + 1) * 128] = zs[j][:, 2:130].T
        if c == 0:
            lr = np.concatenate([zs[3], zs[4], zs[5][:32]], 0)
            shift_prompt[0, :, 3072:] = lr[:, 0:2].T
            shift_sample[0, :, 3072:] = lr[:, 2:130].T
        if "mT" in r:
            mT[c * 128:(c + 1) * 128] = r["mT"]
        if "wkv" in r:
            wk = r["wkv"].reshape(130, 2, 64, 64)
            wkv_prompt[0, :, 2 * c:2 * c + 2] = wk[0:2]
            wkv_sample[0, :, 2 * c:2 * c + 2] = wk[2:130]
    pp = A(p_prompt)[0].reshape(16384, 256)
    psm = A(p_sample)[0].reshape(512, 256)
    if "B" not in _NC_CACHE:
        _NC_CACHE["B"] = build_B()
    in_b = []
    for c in range(8):
        xb = np.zeros((TOKB, 1024), f32); pb = np.zeros((TOKB, 256), f32); mb = np.zeros((1024, TOKB), f32)
        xb[:2048] = xp[c * 2048:(c + 1) * 2048]; xb[2048:2112] = xs[c * 64:(c + 1) * 64]
        pb[:2048] = pp[c * 2048:(c + 1) * 2048]; pb[2048:2112] = psm[c * 64:(c + 1) * 64]
        mb[:, :2048] = mT[:, c * 2048:(c + 1) * 2048]; mb[:, 2048:2112] = mT[:, 16384 + c * 64:16384 + (c + 1) * 64]
        in_b.append(dict(mT=mb, x=xb, p=pb, w_out=A(w_out)[0], gff=A(A(ffn_norm)[0].reshape(8, 128).T),
                         gpl=A(A(ple_norm)[0].reshape(8, 128).T), wr=A(np.concatenate([A(w_grp)[0], A(w_exp)[0]], 1)),
                         br=A(np.concatenate([A(b_grp)[0], A(b_exp)[0]])[None]), w_gate=A(w_gate)[0], w_up=A(w_up)[0],
                         w_down=A(w_down)[0], w_ple_gate=A(w_ple_gate)[0], w_ple_proj=A(w_ple_proj)[0], ident=ident))
    rb = run_bass_kernel_spmd(_NC_CACHE["B"], in_b, core_ids=list(range(8))).results
    y_prompt = np.zeros((16384, 1024), f32); y_sample = np.zeros((512, 1024), f32)
    for c in range(8):
        y = rb[c]["y"]
        y_prompt[c * 2048:(c + 1) * 2048] = y[:2048]
        y_sample[c * 64:(c + 1) * 64] = y[2048:2112]
    return (y_prompt.reshape(2, 8192, 1024), y_sample.reshape(128, 4, 1024), k_prompt, v_prompt, wkv_prompt, shift_prompt,
            k_sample, v_sample, wkv_sample, shift_sample)
```

```python
import numpy as np
from contextlib import ExitStack
import concourse.bass as bass
import concourse.mybir as mybir
from concourse.bass_utils import run_bass_kernel_spmd

F32, BF16, I32 = mybir.dt.float32, mybir.dt.bfloat16, mybir.dt.int32
ALU = mybir.AluOpType
AF = mybir.ActivationFunctionType
AX = mybir.AxisListType

SAME_ENGINE_SYNC = True


class Buf:
    __slots__ = ("name", "w", "r")

    def __init__(self, name):
        self.name = name
        self.w = None
        self.r = {}


class Sched:
    ENG = ("pe", "act", "dve", "pool", "sp")

    def __init__(self, nc, es, plan=None, dry=False):
        self.nc = nc
        self.es = es
        self.es_main = es
        self.dry = dry
        self.phase = "g"
        self.plan = []
        self.pre = {}
        self.po_stack = ExitStack()
        if plan is not None:
            for (name, shape, dt) in plan:
                self.pre[name] = es.enter_context(nc.sbuf_tensor("sb_" + name, list(shape), dt))
        self.prog = {e: [] for e in self.ENG}
        self.count = {e: 0 for e in self.ENG}
        self.seen = {e: {} for e in self.ENG}
        self.sems = {}
        self.dcount = {}
        self.nsem = 0
        for e in ("pe", "act", "dve", "pool"):
            self._sem("E_" + e)
        self.out_tokens = {}

    def _sem(self, name):
        if name not in self.sems:
            self.sems[name] = self.es_main.enter_context(self.nc.semaphore("s%d" % self.nsem))
            self.nsem += 1
            assert self.nsem <= 100, "too many semaphores"
        return self.sems[name]

    def sb(self, name, shape, dt):
        if self.dry:
            if self.phase == "g":
                self.plan.append((name, tuple(shape), dt))
            return self.nc.dram_tensor("dry_" + name, list(shape), dt).ap()
        if self.phase == "g":
            if name in self.pre:
                return self.pre[name]
            return self.es_main.enter_context(self.nc.sbuf_tensor("sb_" + name, list(shape), dt))
        if self.phase == "po":
            return self.po_stack.enter_context(self.nc.sbuf_tensor("sb_" + name, list(shape), dt))
        return self.es_main.enter_context(self.nc.sbuf_tensor("sb_" + name, list(shape), dt))

    def free_po(self):
        if not self.dry:
            self.po_stack.close()
        self.phase = "s"

    def ps(self, name, shape, dt):
        if self.dry:
            return self.nc.dram_tensor("dryp_" + name, list(shape), dt).ap()
        return self.es_main.enter_context(self.nc.psum_tensor("ps_" + name, list(shape), dt))

    def _waits(self, eng, deps):
        need = {}
        for d in deps:
            if d is None:
                continue
            s, v = d
            if need.get(s, 0) < v:
                need[s] = v
        own = "E_" + eng
        for s, v in need.items():
            if s == own and (not SAME_ENGINE_SYNC or eng == "pe"):
                continue
            if self.seen[eng].get(s, 0) >= v:
                continue
            self.seen[eng][s] = v
            sem = self.sems[s]
            self.prog[eng].append(lambda h, sem=sem, v=v: h.wait_ge(sem, v))

    def _deps(self, reads, writes):
        deps = []
        for b in reads:
            deps.append(b.w)
        for b in writes:
            deps.append(b.w)
            deps.extend(b.r.items())
        return deps

    def _mark(self, tok, reads, writes):
        s, v = tok
        for b in reads:
            if b.r.get(s, 0) < v:
                b.r[s] = v
        for b in writes:
            b.w = tok
            b.r = {}

    def op(self, eng, meth, *args, reads=(), writes=(), **kw):
        self._waits(eng, self._deps(reads, writes))
        self.count[eng] += 1
        n = self.count[eng]
        sem = self.sems["E_" + eng]
        self.prog[eng].append(lambda h, meth=meth, args=args, kw=kw, sem=sem: getattr(h, meth)(*args, **kw).then_inc(sem, 1))
        self._mark(("E_" + eng, n), reads, writes)

    def dma(self, q, out=None, in_=None, stream=None, reads=(), writes=(), is_output=False, meth="dma_start", **kw):
        self._waits(q, self._deps(reads, writes))
        sname = "D_" + stream
        sem = self._sem(sname)
        self.dcount[sname] = self.dcount.get(sname, 0) + 16
        v = self.dcount[sname]
        self.prog[q].append(lambda h, out=out, in_=in_, kw=kw, sem=sem, meth=meth: getattr(h, meth)(out=out, in_=in_, **kw).then_inc(sem, 16))
        self._mark((sname, v), reads, writes)
        if is_output:
            self.out_tokens[sname] = v

    def barrier(self):
        latest = {}
        for e in ("pe", "act", "dve", "pool"):
            if self.count[e]:
                latest["E_" + e] = self.count[e]
        latest.update(self.dcount)
        for eng in self.ENG:
            self._waits(eng, list(latest.items()))

    def finish(self):
        for s, v in self.out_tokens.items():
            if self.seen["sp"].get(s, 0) < v:
                sem = self.sems[s]
                self.prog["sp"].append(lambda h, sem=sem, v=v: h.wait_ge(sem, v))

    def emit(self):
        if self.dry:
            return
        self.finish()
        with self.nc.Block() as block:
            @block.tensor
            def _(h):
                for f in self.prog["pe"]:
                    f(h)

            @block.scalar
            def _(h):
                for f in self.prog["act"]:
                    f(h)

            @block.vector
            def _(h):
                for f in self.prog["dve"]:
                    f(h)

            @block.gpsimd
            def _(h):
                for f in self.prog["pool"]:
                    f(h)

            @block.sync
            def _(h):
                for f in self.prog["sp"]:
                    f(h)

NTB = 17
TOKB = NTB * 128
D = 1024
NE = 32
EH = 256
RMS_EPS = 1e-6


def rmsnorm_T(S, name, src_ap, src_buf, gsc, identb, hT, hT_buf, col0, ptr, ptr_buf, scr, cb=()):
    ss, ss_b, sq, sq_b, hb, hb_b = scr
    S.op("act", "activation", out=sq[:], in_=src_ap, func=AF.Square, accum_out=ss[:, 0:1],
         reads=[src_buf], writes=[sq_b, ss_b])
    S.op("dve", "tensor_scalar", out=ss[:, 1:2], in0=ss[:, 0:1], scalar1=1.0 / D, scalar2=RMS_EPS,
                                          op0=ALU.mult, op1=ALU.add, reads=[ss_b], writes=[ss_b])
    S.op("act", "activation", out=ss[:, 3:4], in_=ss[:, 1:2], func=AF.Sqrt, reads=[ss_b], writes=[ss_b])
    S.op("dve", "reciprocal", out=ss[:, 2:3], in_=ss[:, 3:4], reads=[ss_b], writes=[ss_b])
    S.op("act", "activation", out=hb[:], in_=src_ap, func=AF.Copy, scale=ss[:, 2:3],
         reads=[src_buf, ss_b], writes=[hb_b])
    for c in range(8):
        S.op("pe", "transpose", out=ptr[:, c * 128:(c + 1) * 128], in_=hb[:, c * 128:(c + 1) * 128],
                                             identity=identb[:], reads=[hb_b] + list(cb), writes=[ptr_buf])
    for c in range(8):
        S.op("dve", "tensor_scalar", out=hT[:, c, col0:col0 + 128], in0=ptr[:, c * 128:(c + 1) * 128],
                                                  scalar1=gsc[:, c:c + 1], scalar2=None, op0=ALU.mult,
             reads=[ptr_buf] + list(cb), writes=[hT_buf])


def build_B():
    nc = bass.Bass("TRN2", target_bir_lowering=False)
    dr = lambda n, s, dt=F32, kind="ExternalInput": nc.dram_tensor(n, list(s), dt, kind=kind).ap()
    mT_d = dr("mT", [D, TOKB])
    x_d = dr("x", [TOKB, D])
    p_d = dr("p", [TOKB, 256])
    wout_d = dr("w_out", [D, D])
    gff_d = dr("gff", [128, 8])
    gpl_d = dr("gpl", [128, 8])
    wr_d = dr("wr", [D, 36])
    br_d = dr("br", [1, 36])
    wg_d = dr("w_gate", [NE, 128, 8 * EH])
    wu_d = dr("w_up", [NE, 128, 8 * EH])
    wd_d = dr("w_down", [NE, 128, 2 * D])
    wpg_d = dr("w_ple_gate", [D, D])
    wpp_d = dr("w_ple_proj", [256, D])
    id_d = dr("ident", [128, 128])
    y_d = dr("y", [TOKB, D], kind="ExternalOutput")

    with ExitStack() as es:
        S = Sched(nc, es)
        identb = S.sb("identb", [128, 128], BF16); identb_b = Buf("identb")
        wout = S.sb("wout", [128, 8, D], BF16); wout_b = Buf("wout")
        wpg = wout; wpg_b = wout_b
        wpp = S.sb("wpp", [128, 2, D], BF16); wpp_b = Buf("wpp")
        wr = S.sb("wr", [128, 8, 36], BF16); wr_b = Buf("wr")
        brt = S.sb("brt", [128, 36], F32); brt_b = Buf("brt")
        gff = S.sb("gff", [128, 8], F32); gff_b = Buf("gff")
        gpl = S.sb("gpl", [128, 8], F32); gpl_b = Buf("gpl")
        acc = S.sb("acc", [128, NTB, D], F32); acc_b = [Buf("acc%d" % i) for i in range(NTB)]
        h2T = S.sb("h2T", [128, 8, TOKB], BF16); h2T_b = [Buf("h2T%d" % i) for i in range(NTB)]
        comb = S.sb("comb", [128, NTB, NE], F32); comb_b = [Buf("comb%d" % i) for i in range(NTB)]
        hidT = S.sb("hidT", [128, 2, TOKB], BF16); hidT_b = [Buf("hidT%d" % i) for i in range(5)]
        mTs = [S.sb("mTs%d" % i, [128, 8, 128], BF16) for i in range(2)]; mTs_b = [Buf("mTs%d" % i) for i in range(2)]
        xts = [S.sb("xts%d" % i, [128, D], F32) for i in range(2)]; xts_b = [Buf("xts%d" % i) for i in range(2)]
        ss = S.sb("ss", [128, 4], F32); ss_b = Buf("ss")
        sq = S.sb("sq", [128, D], BF16); sq_b = Buf("sq")
        hb = S.sb("hb", [128, D], BF16); hb_b = Buf("hb")
        scr = (ss, ss_b, sq, sq_b, hb, hb_b)
        rt = S.sb("rt", [128, 160], F32); rt_b = Buf("rt")
        wge = [S.sb("wge%d" % i, [128, 8, EH], BF16) for i in range(2)]; wge_b = [Buf("wge%d" % i) for i in range(2)]
        wue = [S.sb("wue%d" % i, [128, 8, EH], BF16) for i in range(2)]; wue_b = [Buf("wue%d" % i) for i in range(2)]
        wde = [S.sb("wde%d" % i, [128, 2, D], BF16) for i in range(2)]; wde_b = [Buf("wde%d" % i) for i in range(2)]
        sil = [S.sb("sil%d" % i, [128, 512], F32) for i in range(2)]; sil_b = [Buf("sil%d" % i) for i in range(2)]
        pts = S.sb("pts", [128, 256], BF16); pts_b = Buf("pts")
        pT = S.sb("pT", [128, 2, 128], BF16); pT_b = Buf("pT")
        hpT = S.sb("hpT", [128, 8, 128], BF16); hpT_b = Buf("hpT")
        sg = S.sb("sg", [128, D], F32); sg_b = Buf("sg")
        yt = [S.sb("yt%d" % i, [128, D], F32) for i in range(2)]; yt_b = [Buf("yt%d" % i) for i in range(2)]
        pA = [S.ps("pA%d" % i, [128, 512], F32) for i in range(2)]; pA_b = [Buf("pA%d" % i) for i in range(2)]
        pB = [S.ps("pB%d" % i, [128, 512], F32) for i in range(2)]; pB_b = [Buf("pB%d" % i) for i in range(2)]
        pC = [S.ps("pC%d" % i, [128, 512], F32) for i in range(2)]; pC_b = [Buf("pC%d" % i) for i in range(2)]
        ptr = S.ps("ptr", [128, 1024], BF16); ptr_b = Buf("ptr")
        pR = S.ps("pR", [128, 512], F32); pR_b = Buf("pR")

        S.dma("pool", out=identb[:], in_=id_d, stream="identb", writes=[identb_b])
        for hh in range(2):
            S.dma("pool",
                out=wout[:, :, hh * 512:(hh + 1) * 512],
                in_=wout_d.rearrange("(c p) n -> p c n", p=128)[:, :, hh * 512:(hh + 1) * 512], stream="wout", writes=[wout_b])
        S.dma("pool", out=wr[:], in_=wr_d.rearrange("(c p) n -> p c n", p=128), stream="wr", writes=[wr_b])
        S.dma("sp", out=brt[:], in_=br_d.partition_broadcast(128), stream="brt", writes=[brt_b])
        S.dma("sp", out=gff[:], in_=gff_d, stream="gff", writes=[gff_b])
        S.dma("sp", out=gpl[:], in_=gpl_d, stream="gpl", writes=[gpl_b])

        def load_expert(e):
            s = e % 2
            S.dma("pool", out=wge[s][:].rearrange("p c n -> p (c n)"), in_=wg_d[e], stream="wge%d" % s, writes=[wge_b[s]])
            S.dma("pool", out=wue[s][:].rearrange("p c n -> p (c n)"), in_=wu_d[e], stream="wue%d" % s, writes=[wue_b[s]])
            S.dma("pool", out=wde[s][:].rearrange("p c n -> p (c n)"), in_=wd_d[e], stream="wde%d" % s, writes=[wde_b[s]])

        for i in range(NTB):
            s = i % 2
            r0 = i * 128
            S.dma("pool", out=mTs[s][:], in_=mT_d.rearrange("(c p) t -> p c t", p=128)[:, :, r0:r0 + 128], stream="mTs%d" % s, writes=[mTs_b[s]])
            S.dma("sp", out=xts[s][:], in_=x_d[r0:r0 + 128, :], stream="xts%d" % s, writes=[xts_b[s]])
            for hh in range(2):
                for c in range(8):
                    S.op("pe", "matmul", pA[hh][:], lhsT=mTs[s][:, c, :],
                                                            rhs=wout[:, c, hh * 512:(hh + 1) * 512],
                                                            start=(c == 0), stop=(c == 7),
                         reads=[mTs_b[s], wout_b], writes=[pA_b[hh]])
                S.op("dve", "tensor_tensor", out=acc[:, i, hh * 512:(hh + 1) * 512], in0=pA[hh][:],
                                                           in1=xts[s][:, hh * 512:(hh + 1) * 512], op=ALU.add,
                     reads=[pA_b[hh], xts_b[s]], writes=[acc_b[i]])
            rmsnorm_T(S, "ffn", acc[:, i, :], acc_b[i], gff, identb, h2T, h2T_b[i], r0, ptr, ptr_b, scr, cb=[identb_b, gff_b])
            for c in range(8):
                S.op("pe", "matmul", pR[:, 0:36], lhsT=h2T[:, c, r0:r0 + 128], rhs=wr[:, c, :],
                                                 start=(c == 0), stop=(c == 7),
                     reads=[h2T_b[i], wr_b], writes=[pR_b])
            L = rt[:, 0:36]
            S.op("dve", "tensor_tensor", out=L, in0=pR[:, 0:36], in1=brt[:], op=ALU.add,
                 reads=[pR_b, brt_b], writes=[rt_b])
            lg = rt[:, 0:4]
            le = rt[:, 4:36]
            mg = rt[:, 36:37]; nmg = rt[:, 37:38]; sgs = rt[:, 38:39]; rsg = rt[:, 39:40]
            oh = rt[:, 40:44]; eg = rt[:, 44:48]
            tmp32 = rt[:, 48:80]
            lsel = rt[:, 80:88]; me = rt[:, 88:89]; nme = rt[:, 89:90]; ee = rt[:, 90:98]
            m1 = rt[:, 98:99]; mk1 = rt[:, 99:107]; ee2 = rt[:, 107:115]; m2 = rt[:, 115:116]; mk2 = rt[:, 116:124]
            den = rt[:, 124:125]; wv = rt[:, 125:126]; sel = rt[:, 126:134]
            R = dict(reads=[rt_b], writes=[rt_b])
            S.op("dve", "tensor_reduce", out=mg, in_=lg, axis=AX.X, op=ALU.max, **R)
            S.op("dve", "tensor_scalar", out=nmg, in0=mg, scalar1=-1.0, scalar2=None, op0=ALU.mult, **R)
            S.op("act", "activation", out=eg, in_=lg, func=AF.Exp, bias=nmg, scale=1.0, accum_out=sgs, **R)
            S.op("dve", "reciprocal", out=rsg, in_=sgs, **R)
            S.op("dve", "tensor_scalar", out=oh, in0=lg, scalar1=mg, scalar2=None, op0=ALU.is_equal, **R)
            S.op("dve", "tensor_tensor", out=tmp32.rearrange("p (g e) -> p g e", g=4),
                                                  in0=le.rearrange("p (g e) -> p g e", g=4),
                                                  in1=oh.unsqueeze(2).broadcast_to([128, 4, 8]), op=ALU.mult, **R)
            S.op("dve", "tensor_reduce", out=lsel, in_=tmp32.rearrange("p (g e) -> p e g", g=4),
                                                  axis=AX.X, op=ALU.add, **R)
            S.op("dve", "tensor_reduce", out=me, in_=lsel, axis=AX.X, op=ALU.max, **R)
            S.op("dve", "tensor_scalar", out=nme, in0=me, scalar1=-1.0, scalar2=None, op0=ALU.mult, **R)
            S.op("act", "activation", out=ee, in_=lsel, func=AF.Exp, bias=nme, scale=1.0, **R)
            S.op("dve", "tensor_reduce", out=m1, in_=ee, axis=AX.X, op=ALU.max, **R)
            S.op("dve", "tensor_scalar", out=mk1, in0=ee, scalar1=m1, scalar2=None, op0=ALU.is_equal, **R)
            S.op("dve", "scalar_tensor_tensor", out=ee2, in0=mk1, scalar=-2.0, in1=ee, op0=ALU.mult, op1=ALU.add, **R)
            S.op("dve", "tensor_reduce", out=m2, in_=ee2, axis=AX.X, op=ALU.max, **R)
            S.op("dve", "tensor_scalar", out=mk2, in0=ee2, scalar1=m2, scalar2=None, op0=ALU.is_equal, **R)
            S.op("dve", "tensor_tensor", out=den, in0=m1, in1=m2, op=ALU.add, **R)
            S.op("dve", "reciprocal", out=wv, in_=den, **R)
            S.op("dve", "tensor_tensor", out=wv, in0=wv, in1=rsg, op=ALU.mult, **R)
            S.op("dve", "tensor_tensor", out=mk1, in0=mk1, in1=mk2, op=ALU.add, **R)
            S.op("dve", "scalar_tensor_tensor", out=sel, in0=mk1, scalar=wv, in1=ee, op0=ALU.mult, op1=ALU.mult, **R)
            S.op("dve", "tensor_tensor", out=comb[:, i, :].rearrange("p (g e) -> p g e", g=4),
                                                  in0=oh.unsqueeze(2).broadcast_to([128, 4, 8]),
                                                  in1=sel.unsqueeze(1).broadcast_to([128, 4, 8]), op=ALU.mult,
                 reads=[rt_b], writes=[comb_b[i]])

        load_expert(0)
        tgs = [(0, 512), (512, 512), (1024, 512), (1536, 512), (2048, 128)]
        for e in range(NE):
            s = e % 2
            if e + 1 < NE:
                load_expert(e + 1)
            for gi, (t0, n) in enumerate(tgs):
                tiles = list(range(t0 // 128, (t0 + n) // 128))
                for fc in range(2):
                    pg, pu = pC[fc], pB[fc]
                    for c in range(8):
                        S.op("pe", "matmul", pg[:, 0:n], lhsT=wge[s][:, c, fc * 128:(fc + 1) * 128],
                                                                       rhs=h2T[:, c, t0:t0 + n], start=(c == 0), stop=(c == 7),
                             reads=[wge_b[s]] + [h2T_b[t] for t in tiles], writes=[pC_b[fc]])
                    for c in range(8):
                        S.op("pe", "matmul", pu[:, 0:n], lhsT=wue[s][:, c, fc * 128:(fc + 1) * 128],
                                                                       rhs=h2T[:, c, t0:t0 + n], start=(c == 0), stop=(c == 7),
                             reads=[wue_b[s]] + [h2T_b[t] for t in tiles], writes=[pB_b[fc]])
                    S.op("act", "activation", out=sil[fc][:, 0:n], in_=pg[:, 0:n], func=AF.Silu,
                         reads=[pC_b[fc]], writes=[sil_b[fc]])
                    S.op("dve", "tensor_tensor", out=hidT[:, fc, t0:t0 + n], in0=sil[fc][:, 0:n],
                                                                      in1=pu[:, 0:n], op=ALU.mult,
                         reads=[sil_b[fc], pB_b[fc]], writes=[hidT_b[gi]])
                for i in tiles:
                    r0 = i * 128
                    for hh in range(2):
                        for fc in range(2):
                            S.op("pe", "matmul", pA[hh][:], lhsT=hidT[:, fc, r0:r0 + 128],
                                                                             rhs=wde[s][:, fc, hh * 512:(hh + 1) * 512],
                                                                             start=(fc == 0), stop=(fc == 1),
                                 reads=[hidT_b[gi], wde_b[s]], writes=[pA_b[hh]])
                        S.op("dve", "scalar_tensor_tensor",
                            out=acc[:, i, hh * 512:(hh + 1) * 512], in0=pA[hh][:], scalar=comb[:, i, e:e + 1],
                            in1=acc[:, i, hh * 512:(hh + 1) * 512], op0=ALU.mult, op1=ALU.add,
                            reads=[pA_b[hh], comb_b[i], acc_b[i]], writes=[acc_b[i]])

        for hh in range(2):
            S.dma("pool",
                out=wpg[:, :, hh * 512:(hh + 1) * 512],
                in_=wpg_d.rearrange("(c p) n -> p c n", p=128)[:, :, hh * 512:(hh + 1) * 512], stream="wout", writes=[wpg_b])
        S.dma("pool", out=wpp[:], in_=wpp_d.rearrange("(c p) n -> p c n", p=128), stream="wpp", writes=[wpp_b])
        for i in range(NTB):
            s = i % 2
            r0 = i * 128
            S.dma("pool", out=pts[:], in_=p_d[r0:r0 + 128, :], stream="pts", writes=[pts_b])
            rmsnorm_T(S, "ple", acc[:, i, :], acc_b[i], gpl, identb, hpT, hpT_b, 0, ptr, ptr_b, scr, cb=[identb_b, gpl_b])
            for c in range(2):
                S.op("pe", "transpose", out=ptr[:, c * 128:(c + 1) * 128], in_=pts[:, c * 128:(c + 1) * 128],
                                                     identity=identb[:], reads=[pts_b, identb_b], writes=[ptr_b])
            S.op("dve", "tensor_copy", out=pT[:].rearrange("p c t -> p (c t)"), in_=ptr[:, 0:256],
                 reads=[ptr_b], writes=[pT_b])
            for hh in range(2):
                for c in range(8):
                    S.op("pe", "matmul", pC[hh][:], lhsT=hpT[:, c, :], rhs=wpg[:, c, hh * 512:(hh + 1) * 512],
                                                            start=(c == 0), stop=(c == 7),
                         reads=[hpT_b, wpg_b], writes=[pC_b[hh]])
                for c in range(2):
                    S.op("pe", "matmul", pB[hh][:], lhsT=pT[:, c, :], rhs=wpp[:, c, hh * 512:(hh + 1) * 512],
                                                            start=(c == 0), stop=(c == 1),
                         reads=[pT_b, wpp_b], writes=[pB_b[hh]])
                S.op("act", "activation", out=sg[:, hh * 512:(hh + 1) * 512], in_=pC[hh][:], func=AF.Sigmoid,
                     reads=[pC_b[hh]], writes=[sg_b])
                S.op("dve", "tensor_tensor", out=sg[:, hh * 512:(hh + 1) * 512], in0=sg[:, hh * 512:(hh + 1) * 512],
                                                           in1=pB[hh][:], op=ALU.mult,
                     reads=[sg_b, pB_b[hh]], writes=[sg_b])
            S.op("dve", "tensor_tensor", out=yt[s][:], in0=sg[:], in1=acc[:, i, :], op=ALU.add,
                 reads=[sg_b, acc_b[i]], writes=[yt_b[s]])
            S.dma("sp", out=y_d[r0:r0 + 128, :], in_=yt[s][:], stream="yt%d" % s, reads=[yt_b[s]], is_output=True)
        S.emit()
    return nc
import math

NTOK_A = 16384 + 512
NG_A = NTOK_A // 512
WCOLS = 1312
QK_EPS = 1e-6


def build_A(n_groups=NG_A, do_attn=False, do_rwkv=False, groups=None):
    plan = _build_A(n_groups, do_attn, do_rwkv, groups, None)
    return _build_A(n_groups, do_attn, do_rwkv, groups, plan)


def _build_A(n_groups, do_attn, do_rwkv, groups, plan):
    nc = bass.Bass("TRN2", target_bir_lowering=False)
    dr = lambda n, s, dt=F32, kind="ExternalInput": nc.dram_tensor(n, list(s), dt, kind=kind).ap()
    x_d = dr("x", [NTOK_A, D])
    w_d = dr("w", [D, WCOLS])
    gat_d = dr("gat", [128, 8])
    gqk_d = dr("gqk", [128, 2])
    id_d = dr("ident", [128, 128])
    bo_d = dr("blockones", [128, 128])
    kout_d = dr("kout", [NTOK_A, 128], kind="ExternalOutput")
    vout_d = dr("vout", [NTOK_A, 128], kind="ExternalOutput")
    zs_d = dr("zs", [6, 128, 130], kind="ExternalOutput")

    with ExitStack() as es:
        S = Sched(nc, es, plan=plan, dry=(plan is None))
        identb = S.sb("identb", [128, 128], BF16); identb_b = Buf("identb")
        identf = S.sb("identf", [128, 128], F32); identf_b = Buf("identf")
        bones = S.sb("bones", [128, 128], F32); bones_b = Buf("bones")
        W = S.sb("W", [128, 8, WCOLS], BF16); W_b = Buf("W")
        gat = S.sb("gat", [128, 8], F32); gat_b = Buf("gat")
        gqk = S.sb("gqk", [128, 2], F32); gqk_b = Buf("gqk")
        xts = [S.sb("xts", [128, D], F32)] * 2; xts_b = [Buf("xts")] * 2
        ss = S.sb("ss", [128, 4], F32); ss_b = Buf("ss")
        hb = S.sb("hb", [128, D], BF16); hb_b = Buf("hb")
        sq, sq_b = hb, hb_b
        scr = (ss, ss_b, sq, sq_b, hb, hb_b)
        hT = [S.sb("hT", [128, 8, 512], BF16)] * 2; hT_b = [Buf("hT")] * 2
        ZN = 10
        zt = [S.sb("zt%d" % i, [128, 513], F32) for i in range(ZN)]; zt_b = [Buf("zt%d" % i) for i in range(ZN)]
        sqf = S.sb("sqf", [128, 512], F32); sqf_b = Buf("sqf")
        rstd = S.sb("rstd", [128, 512], F32); rstd_b = Buf("rstd")
        kn = S.sb("kn", [128, 512], F32); kn_b = Buf("kn")
        qn = S.sb("qn", [128, 512], BF16); qn_b = Buf("qn")
        kT_o = [S.sb("kTo", [128, 512], F32)] * 2; kT_o_b = [Buf("kTo")] * 2
        v_o = [S.sb("vo", [128, 512], F32)] * 2; v_o_b = [Buf("vo")] * 2
        zs = S.sb("zs", [128, 6, 130], F32); zs_b = Buf("zs")
        pP = [S.ps("pP%d" % i, [128, 512], F32) for i in range(3)]; pP_b = [Buf("pP%d" % i) for i in range(3)]
        ptr = S.ps("ptr", [128, 1024], BF16); ptr_b = Buf("ptr")
        pS, pS_b = pP[2], pP_b[2]
        pV, pV_b = pP[1], pP_b[1]

        S.op("pool", "memset", zs[:].rearrange("p a b -> p (a b)"), 0.0, writes=[zs_b])
        S.dma("pool", out=identb[:], in_=id_d, stream="identb", writes=[identb_b])
        S.dma("sp", out=identf[:], in_=id_d, stream="identf", writes=[identf_b])
        S.dma("sp", out=bones[:], in_=bo_d, stream="bones", writes=[bones_b])
        S.dma("sp", out=gat[:], in_=gat_d, stream="gat", writes=[gat_b])
        S.dma("sp", out=gqk[:], in_=gqk_d, stream="gqk", writes=[gqk_b])
        for c in range(8):
            S.dma("pool", out=W[:, c, :], in_=w_d[c * 128:(c + 1) * 128, :], stream="W", writes=[W_b])

        rwc_d = dr("rwc", [128, 16])
        w2a2_d = dr("w2a2", [128, 128])
        g2c_d = dr("g2c", [2, 128, 128])
        maskAR_d = dr("maskAR", [128, 256])
        maskSL_d = dr("maskSL", [128, 128])
        scanm_d = dr("scanm", [128, 512])
        wkv_d = dr("wkv", [130, 128, 64], kind="ExternalOutput")
        mT_d = dr("mT", [128, NTOK_A], kind="ExternalOutput")
        cst = lambda name, shape: (S.sb(name, shape, F32), Buf(name))
        rwc, rwc_b = cst("rwc", [128, 16]); w2a2, w2a2_b = cst("w2a2", [128, 128]); g2c, g2c_b = cst("g2c", [128, 2, 128])
        S.phase = "po"
        maskAR, maskAR_b = cst("maskAR", [128, 256]); maskSL, maskSL_b = cst("maskSL", [128, 128]); scanm, scanm_b = cst("scanm", [128, 512])
        S.phase = "g"
        S.dma("sp", out=rwc[:], in_=rwc_d, stream="rwc", writes=[rwc_b])
        S.dma("sp", out=w2a2[:], in_=w2a2_d, stream="w2a2", writes=[w2a2_b])
        S.dma("sp", out=g2c[:], in_=g2c_d.rearrange("k p n -> p k n"), stream="g2c", writes=[g2c_b])
        S.dma("sp", out=maskAR[:], in_=maskAR_d, stream="maskAR", writes=[maskAR_b])
        S.dma("sp", out=maskSL[:], in_=maskSL_d, stream="maskSL", writes=[maskSL_b])
        S.dma("sp", out=scanm[:], in_=scanm_d, stream="scanm", writes=[scanm_b])
        zsh = []; zsh_b = []
        for k in range(6):
            t_, b_ = cst("zsh%d" % k, [128, 512]); zsh.append(t_); zsh_b.append(b_)
        names = ["tmpA", "th", "ld", "av", "sg1", "sg2", "gv", "kk", "kf", "kka", "Lc", "eg"]
        WK = {}
        for nm in names:
            WK[nm] = cst("rk_" + nm, [128, 512])
        WK["dd"] = WK["th"]; WK["yo"] = WK["sg1"]; WK["sgb"] = WK["sg2"]; WK["yT"] = WK["Lc"]
        WK["einv"] = (zsh[3], zsh_b[3]); WK["eprev"] = (zsh[4], zsh_b[4])
        S.phase = "po"
        AR, AR_b = cst("AR", [128, 8, 256])
        BT, BT_b = cst("BT", [128, 8, 128]); KT, KT_b = cst("KT", [128, 8, 128]); VT, VT_b = cst("VT", [128, 8, 128])
        BbT, BbT_b = cst("BbT", [128, 8, 128]); KbT, KbT_b = cst("KbT", [128, 8, 128])
        for t_, b_ in ((AR, AR_b), (BT, BT_b), (KT, KT_b), (VT, VT_b), (BbT, BbT_b), (KbT, KbT_b)):
            S.op("pool", "memset", t_[:].rearrange("p a b -> p (a b)"), 0.0, writes=[b_])
        CS = []
        for q_ in range(2):
            CS.append(dict(NR=cst("NR%d" % q_, [128, 256]), AK=cst("AK%d" % q_, [128, 256]),
                           Ab=[cst("Ab%d_%d" % (q_, i), [128, 128]) for i in range(2)], Nb=[cst("Nb%d_%d" % (q_, i), [128, 128]) for i in range(2)],
                           A0=cst("A0_%d" % q_, [128, 128]), Mi=cst("Mi%d" % q_, [128, 128]), TR=cst("TR%d" % q_, [128, 384])))
        Xs, Xs_b = cst("Xs", [128, 128]); Us, Us_b = cst("Us", [128, 128])
        Wst, Wst_b = cst("Wst", [128, 128]); wkvo, wkvo_b = cst("wkvo", [128, 64])
        S.phase = "g"
        mTo = [cst("mTo", [128, 512])] * 2
        prr = [0]

        def pnext():
            i = prr[0] % 3; prr[0] += 1
            return pP[i], pP_b[i]

        def mm(out, ob, lhsT, lb, rhs, rb, start=True, stop=True, skip=False):
            if skip:
                S.op("pe", "matmul", out, lhsT=lhsT, rhs=rhs, start=start, stop=stop, skip_group_check=True, reads=[lb, rb], writes=[ob])
            else:
                S.op("pe", "matmul", out, lhsT=lhsT, rhs=rhs, start=start, stop=stop, reads=[lb, rb], writes=[ob])

        def rwkv_group(g, sample=False):
            t0 = g * 512
            tmpA, tmpA_b = WK["tmpA"]
            yT, yT_b = WK["yT"]
            if sample:
                sample_shift()
            for k, zi in enumerate([] if sample else [2, 3, 4, 7, 8, 9]):
                rows = 32 if zi == 9 else 128
                S.op("dve", "tensor_tensor", out=tmpA[0:rows, :], in0=zt[zi][0:rows, 0:512], in1=zt[zi][0:rows, 1:513], op=ALU.subtract,
                     reads=[zt_b[zi]], writes=[tmpA_b])
                S.op("dve", "scalar_tensor_tensor", out=zsh[k][0:rows, :], in0=tmpA[0:rows, :], scalar=rwc[0:rows, k:k + 1],
                     in1=zt[zi][0:rows, 1:513], op0=ALU.mult, op1=ALU.add, reads=[tmpA_b, zt_b[zi], rwc_b], writes=[zsh_b[k]])
            r_, r_b = zsh[0], zsh_b[0]; k_, k_b = zsh[1], zsh_b[1]; v_, v_b = zsh[2], zsh_b[2]
            th, th_b = WK["th"]; ld, ld_b = WK["ld"]; av, av_b = WK["av"]; sg1, sg1_b = WK["sg1"]; sg2, sg2_b = WK["sg2"]
            gv, gv_b = WK["gv"]; kk, kk_b = WK["kk"]; kf, kf_b = WK["kf"]; kka, kka_b = WK["kka"]; Lc, Lc_b = WK["Lc"]
            eg, eg_b = WK["eg"]; einv, einv_b = WK["einv"]; eprev, eprev_b = WK["eprev"]
            S.op("act", "activation", out=th[0:64, :], in_=zsh[3][0:64, :], func=AF.Tanh, reads=[zsh_b[3]], writes=[th_b])
            p, pb = pnext()
            mm(p[:], pb, w2a2[0:64, :], w2a2_b, th[0:64, :], th_b)
            S.op("act", "activation", out=ld[:], in_=p[:], func=AF.Sigmoid, bias=rwc[:, 6:7], scale=1.0, reads=[pb, rwc_b], writes=[ld_b])
            S.op("dve", "tensor_scalar", out=ld[:], in0=ld[:], scalar1=-math.exp(-0.5), scalar2=None, op0=ALU.mult, reads=[ld_b], writes=[ld_b])
            p, pb = pnext()
            mm(p[:], pb, w2a2[64:128, :], w2a2_b, zsh[3][64:128, :], zsh_b[3])
            S.op("act", "activation", out=av[:], in_=p[:], func=AF.Sigmoid, bias=rwc[:, 7:8], scale=1.0, reads=[pb, rwc_b], writes=[av_b])
            S.op("act", "activation", out=sg1[:], in_=zsh[4][:], func=AF.Sigmoid, reads=[zsh_b[4]], writes=[sg1_b])
            S.op("act", "activation", out=sg2[0:32, :], in_=zsh[5][0:32, :], func=AF.Sigmoid, reads=[zsh_b[5]], writes=[sg2_b])
            p, pb = pnext()
            mm(p[:], pb, g2c[:, 0, :], g2c_b, sg1[:], sg1_b, True, False)
            mm(p[:], pb, g2c[0:32, 1, :], g2c_b, sg2[0:32, :], sg2_b, False, True)
            S.op("act", "copy", out=gv[:], in_=p[:], reads=[pb], writes=[gv_b])
            S.op("dve", "tensor_scalar", out=kk[:], in0=k_[:], scalar1=rwc[:, 8:9], scalar2=None, op0=ALU.mult, reads=[k_b, rwc_b], writes=[kk_b])
            S.op("act", "activation", out=sqf[:], in_=kk[:], func=AF.Square, reads=[kk_b], writes=[sqf_b])
            mm(pS[:], pS_b, bones[:], bones_b, sqf[:], sqf_b)
            S.op("dve", "tensor_scalar", out=rstd[:], in0=pS[:], scalar1=1e-24, scalar2=None, op0=ALU.max, reads=[pS_b], writes=[rstd_b])
            S.op("act", "activation", out=rstd[:], in_=rstd[:], func=AF.Sqrt, reads=[rstd_b], writes=[rstd_b])
            S.op("dve", "reciprocal", out=rstd[:], in_=rstd[:], reads=[rstd_b], writes=[rstd_b])
            S.op("dve", "tensor_tensor", out=kk[:], in0=kk[:], in1=rstd[:], op=ALU.mult, reads=[kk_b, rstd_b], writes=[kk_b])
            S.op("dve", "tensor_scalar", out=kf[:], in0=av[:], scalar1=-1.0, scalar2=rwc[:, 9:10], op0=ALU.add, op1=ALU.mult,
                 reads=[av_b, rwc_b], writes=[kf_b])
            S.op("dve", "scalar_tensor_tensor", out=kf[:], in0=kf[:], scalar=1.0, in1=k_[:], op0=ALU.add, op1=ALU.mult,
                 reads=[kf_b, k_b], writes=[kf_b])
            S.op("pool", "tensor_tensor", out=kka[:], in0=kk[:], in1=av[:], op=ALU.mult, reads=[kk_b, av_b], writes=[kka_b])
            if sample:
                yield from sample_scan()
                yield
            else:
                S.op("dve", "tensor_tensor_scan", out=Lc[:], data0=scanm[:], data1=ld[:], initial=0.0, op0=ALU.mult, op1=ALU.add,
                     reads=[scanm_b, ld_b], writes=[Lc_b])
                S.op("act", "activation", out=eg[:], in_=Lc[:], func=AF.Exp, reads=[Lc_b], writes=[eg_b])
                S.op("act", "activation", out=einv[:], in_=Lc[:], func=AF.Exp, scale=-1.0, reads=[Lc_b], writes=[einv_b])
                S.op("pool", "tensor_tensor", out=tmpA[:], in0=Lc[:], in1=ld[:], op=ALU.subtract, reads=[Lc_b, ld_b], writes=[tmpA_b])
                S.op("act", "activation", out=eprev[:], in_=tmpA[:], func=AF.Exp, reads=[tmpA_b], writes=[eprev_b])
                c3 = lambda ap: ap.rearrange("p (n t) -> p n t", t=64)
                for hh in range(2):
                    R = slice(hh * 64, (hh + 1) * 64)
                    C = slice(hh * 64, (hh + 1) * 64)
                    C2 = slice(128 + hh * 64, 128 + (hh + 1) * 64)
                    S.op("dve", "scalar_tensor_tensor", out=AR[R, :, C], in0=c3(kk[R, :]), scalar=-1.0, in1=c3(eprev[R, :]),
                         op0=ALU.mult, op1=ALU.mult, reads=[kk_b, eprev_b], writes=[AR_b])
                    S.op("pool", "tensor_tensor", out=AR[R, :, C2], in0=c3(r_[R, :]), in1=c3(eg[R, :]), op=ALU.mult,
                         reads=[r_b, eg_b], writes=[AR_b])
                    S.op("dve", "tensor_tensor", out=BT[R, :, C], in0=c3(kka[R, :]), in1=c3(einv[R, :]), op=ALU.mult,
                         reads=[kka_b, einv_b], writes=[BT_b])
                    S.op("pool", "tensor_tensor", out=KT[R, :, C], in0=c3(kf[R, :]), in1=c3(einv[R, :]), op=ALU.mult,
                         reads=[kf_b, einv_b], writes=[KT_b])
                    S.op("pool", "tensor_copy", out=VT[R, :, C], in_=c3(v_[R, :]), reads=[v_b], writes=[VT_b])
                    gCb = c3(eg[R, :])[:, :, 63:64].broadcast_to([64, 8, 64])
                    S.op("dve", "tensor_tensor", out=BbT[R, :, C], in0=BT[R, :, C], in1=gCb, op=ALU.mult, reads=[BT_b, eg_b], writes=[BbT_b])
                    S.op("pool", "tensor_tensor", out=KbT[R, :, C], in0=KT[R, :, C], in1=gCb, op=ALU.mult, reads=[KT_b, eg_b], writes=[KbT_b])
                if g in (0, 16):
                    S.op("pool", "memset", Wst[:], 0.0, writes=[Wst_b])
                yT, yT_b = WK["yT"]
                def prep_gen(n, cs):
                    NR, NR_b = cs["NR"]; AK, AK_b = cs["AK"]; Ab = cs["Ab"]; Nb = cs["Nb"]; A0, A0_b = cs["A0"]; Mi, Mi_b = cs["Mi"]; TR, TR_b = cs["TR"]
                    p, pb = pnext()
                    mm(p[:, 0:256], pb, BT[:, n, :], BT_b, AR[:, n, :], AR_b)
                    S.op("dve", "tensor_tensor", out=NR[:], in0=p[:, 0:256], in1=maskAR[:], op=ALU.mult, reads=[pb, maskAR_b], writes=[NR_b])
                    p, pb = pnext()
                    mm(p[:, 0:256], pb, KT[:, n, :], KT_b, AR[:, n, :], AR_b)
                    S.op("dve", "tensor_tensor", out=AK[:], in0=p[:, 0:256], in1=maskAR[:], op=ALU.mult, reads=[pb, maskAR_b], writes=[AK_b])
                    p, pb = pnext()
                    mm(p[:, 0:128], pb, AR[:, n, 0:128], AR_b, BT[:, n, :], BT_b)
                    S.op("dve", "tensor_tensor", out=A0[:], in0=p[:, 0:128], in1=maskSL[:], op=ALU.mult, reads=[pb, maskSL_b], writes=[A0_b])
                    S.op("pool", "tensor_tensor", out=Mi[:], in0=NR[:, 0:128], in1=identf[:], op=ALU.add, reads=[NR_b, identf_b], writes=[Mi_b])
                    yield
                    Nk, Nk_b, Ak, Ak_b = NR[:, 0:128], NR_b, A0[:], A0_b
                    for lvl in range(1, 6):
                        An, An_b = Ab[lvl % 2]
                        p, pb = pnext()
                        mm(p[:, 0:128], pb, Nk, Nk_b, Ak, Ak_b)
                        S.op("act", "copy", out=An[:], in_=p[:, 0:128], reads=[pb], writes=[An_b])
                        if lvl <= 4:
                            Nn, Nn_b = Nb[lvl % 2]
                            p, pb = pnext()
                            mm(p[:, 0:128], pb, Ak, Ak_b, Nk, Nk_b)
                            S.op("dve", "tensor_copy", out=Nn[:], in_=p[:, 0:128], reads=[pb], writes=[Nn_b])
                        p, pb = pnext()
                        mm(p[:, 0:128], pb, An[:], An_b, Mi[:], Mi_b)
                        S.op("dve", "tensor_tensor", out=Mi[:], in0=p[:, 0:128], in1=Mi[:], op=ALU.add, reads=[pb, Mi_b], writes=[Mi_b])
                        yield
                        Ak, Ak_b = An[:], An_b
                        if lvl <= 4:
                            Nk, Nk_b = Nn[:], Nn_b
                    p, pb = pnext()
                    for j, (T_, Tb_) in enumerate(((VT, VT_b), (BbT, BbT_b), (KbT, KbT_b))):
                        S.op("pe", "transpose", out=p[:, j * 128:(j + 1) * 128], in_=T_[:, n, :], identity=identf[:],
                             reads=[Tb_, identf_b], writes=[pb])
                    S.op("act", "copy", out=TR[:], in_=p[:, 0:384], reads=[pb], writes=[TR_b])
                    Vb, Bb, Kb = TR[:, 0:128], TR[:, 128:256], TR[:, 256:384]
                    yield

                def chain(n, cs):
                    NR, NR_b = cs["NR"]; AK, AK_b = cs["AK"]; Ab = cs["Ab"]; Nb = cs["Nb"]; A0, A0_b = cs["A0"]; Mi, Mi_b = cs["Mi"]; TR, TR_b = cs["TR"]
                    Vb, Bb, Kb = TR[:, 0:128], TR[:, 128:256], TR[:, 256:384]
                    p, pb = pnext()
                    mm(p[:, 0:128], pb, AR[:, n, 0:128], AR_b, Wst[:], Wst_b, True, False)
                    mm(p[:, 0:128], pb, AK[:, 0:128], AK_b, Vb, TR_b, False, True)
                    S.op("act", "copy", out=Xs[:], in_=p[:, 0:128], reads=[pb], writes=[Xs_b])
                    p, pb = pnext()
                    mm(p[:, 0:128], pb, Mi[:], Mi_b, Xs[:], Xs_b)
                    S.op("dve", "tensor_copy", out=Us[:], in_=p[:, 0:128], reads=[pb], writes=[Us_b])
                    p, pb = pnext()
                    mm(p[:, 0:128], pb, Wst[:], Wst_b, AR[:, n, 128:256], AR_b, True, False)
                    mm(p[:, 0:128], pb, Us[:], Us_b, NR[:, 128:256], NR_b, False, False)
                    mm(p[:, 0:128], pb, Vb, TR_b, AK[:, 128:256], AK_b, False, True)
                    S.op("act", "copy", out=yT[0:64, n * 64:(n + 1) * 64], in_=p[0:64, 0:64], reads=[pb], writes=[yT_b])
                    S.op("act", "copy", out=yT[64:128, n * 64:(n + 1) * 64], in_=p[64:128, 64:128], reads=[pb], writes=[yT_b])
                    p, pb = pnext()
                    mm(p[:, 0:128], pb, Bb, TR_b, Us[:], Us_b, True, False)
                    mm(p[:, 0:128], pb, Kb, TR_b, Vb, TR_b, False, True)
                    for hh in range(2):
                        R = slice(hh * 64, (hh + 1) * 64)
                        S.op("dve", "scalar_tensor_tensor", out=Wst[R, :], in0=Wst[R, :], scalar=eg[R, n * 64 + 63:n * 64 + 64], in1=p[R, 0:128],
                             op0=ALU.mult, op1=ALU.add, reads=[Wst_b, eg_b, pb], writes=[Wst_b])

                for n0 in range(0, 8, 2):
                    gens = [prep_gen(n0, CS[0]), prep_gen(n0 + 1, CS[1])]
                    while gens:
                        for ge in list(gens):
                            try:
                                next(ge)
                            except StopIteration:
                                gens.remove(ge)
                        yield
                    chain(n0, CS[0])
                    yield
                    chain(n0 + 1, CS[1])
                    yield
            dd, dd_b = WK["dd"]; yo, yo_b = WK["yo"]; sgb, sgb_b = WK["sgb"]
            mm(pS[:], pS_b, bones[:], bones_b, yT[:], yT_b)
            S.op("dve", "scalar_tensor_tensor", out=dd[:], in0=pS[:], scalar=-1.0 / 64, in1=yT[:], op0=ALU.mult, op1=ALU.add,
                 reads=[pS_b, yT_b], writes=[dd_b])
            S.op("act", "activation", out=sqf[:], in_=dd[:], func=AF.Square, reads=[dd_b], writes=[sqf_b])
            mm(pS[:], pS_b, bones[:], bones_b, sqf[:], sqf_b)
            S.op("dve", "tensor_scalar", out=rstd[:], in0=pS[:], scalar1=1.0 / 64, scalar2=64e-5, op0=ALU.mult, op1=ALU.add,
                 reads=[pS_b], writes=[rstd_b])
            S.op("act", "activation", out=rstd[:], in_=rstd[:], func=AF.Sqrt, reads=[rstd_b], writes=[rstd_b])
            S.op("dve", "reciprocal", out=rstd[:], in_=rstd[:], reads=[rstd_b], writes=[rstd_b])
            S.op("dve", "tensor_tensor", out=dd[:], in0=dd[:], in1=rstd[:], op=ALU.mult, reads=[dd_b, rstd_b], writes=[dd_b])
            S.op("dve", "tensor_scalar", out=yo[:], in0=dd[:], scalar1=rwc[:, 11:12], scalar2=rwc[:, 12:13], op0=ALU.mult, op1=ALU.add,
                 reads=[dd_b, rwc_b], writes=[yo_b])
            S.op("dve", "scalar_tensor_tensor", out=tmpA[:], in0=r_[:], scalar=rwc[:, 10:11], in1=kf[:], op0=ALU.mult, op1=ALU.mult,
                 reads=[r_b, kf_b, rwc_b], writes=[tmpA_b])
            mm(pS[:], pS_b, bones[:], bones_b, tmpA[:], tmpA_b)
            S.op("dve", "tensor_tensor", out=dd[:], in0=pS[:], in1=v_[:], op=ALU.mult, reads=[pS_b, v_b], writes=[dd_b])
            S.op("dve", "tensor_tensor", out=yo[:], in0=yo[:], in1=dd[:], op=ALU.add, reads=[yo_b, dd_b], writes=[yo_b])
            S.op("dve", "tensor_tensor", out=yo[:], in0=yo[:], in1=gv[:], op=ALU.mult, reads=[yo_b, gv_b], writes=[yo_b])
            S.op("act", "activation", out=sgb[:], in_=zt[6][:, 1:513], func=AF.Sigmoid, reads=[zt_b[6]], writes=[sgb_b])
            mo, mo_b = mTo[g % 2]
            S.op("dve", "tensor_tensor", out=mo[:], in0=yo[:], in1=sgb[:], op=ALU.mult, reads=[yo_b, sgb_b], writes=[mo_b])
            if (not sample) and g in (15, 31):
                p, pb = pnext()
                S.op("pe", "transpose", out=p[:, 0:128], in_=Wst[:], identity=identf[:], reads=[Wst_b, identf_b], writes=[pb])
                S.op("act", "copy", out=wkvo[0:64, :], in_=p[0:64, 0:64], reads=[pb], writes=[wkvo_b])
                S.op("act", "copy", out=wkvo[64:128, :], in_=p[64:128, 64:128], reads=[pb], writes=[wkvo_b])
                S.dma("sp", out=wkv_d[0 if g == 15 else 1], in_=wkvo[:], stream="wkvo", reads=[wkvo_b], is_output=True)
        ATT_SCALE = 0.125
        LAMBDA_INIT = 0.2
        rbc_d = dr("rbc", [33, 1])
        b31_d = dr("b31", [1, 1])
        oh_d = dr("bucket_onehot", [33, 383])
        lam_d = dr("lamv", [4, 64])
        sub_d = dr("subg", [128, 1])
        tab_d = dr("tab_scratch", [1, 384], kind="Internal")
        b31c, b31c_b = cst("b31c", [128, 1]); DE, DE_b = cst("DE", [128, 256]); subg, subg_b = cst("subg", [128, 1])
        S.phase = "po"
        rbc, rbc_b = cst("rbc", [33, 1]); oh, oh_b = cst("oh", [33, 383]); tabr, tabr_b = cst("tabr", [1, 384])
        S.phase = "g"
        lamt, lamt_b = cst("lamt", [128, 4, 64]); lamw, lamw_b = cst("lamw", [128, 8])
        tab_b = Buf("tab_dram")
        S.dma("sp", out=rbc[:], in_=rbc_d, stream="rbc", writes=[rbc_b])
        S.dma("sp", out=b31c[:], in_=b31_d.partition_broadcast(128), stream="b31c", writes=[b31c_b])
        S.dma("sp", out=oh[:], in_=oh_d, stream="oh", writes=[oh_b])
        S.dma("sp", out=subg[:], in_=sub_d, stream="subg", writes=[subg_b])
        S.dma("sp", out=lamt[:].rearrange("p a b -> p (a b)"), in_=lam_d.rearrange("a b -> (a b)").partition_broadcast(128),
              stream="lamt", writes=[lamt_b])
        S.op("dve", "tensor_scalar", out=rbc[0:32, :], in0=rbc[0:32, :], scalar1=b31c[0:32, 0:1], scalar2=8.0, op0=ALU.subtract, op1=ALU.mult,
             reads=[rbc_b, b31c_b], writes=[rbc_b])
        S.op("dve", "memset", rbc[32:33, :], -240000.0, writes=[rbc_b])
        p, pb = pnext()
        mm(p[0:1, 0:383], pb, rbc[:, :], rbc_b, oh[:, :], oh_b)
        S.op("dve", "tensor_copy", out=tabr[:, 0:383], in_=p[0:1, 0:383], reads=[pb], writes=[tabr_b])
        S.dma("sp", out=tab_d[:, 0:383], in_=tabr[:, 0:383], stream="tabw", reads=[tabr_b], writes=[tab_b])
        for kq in range(128):
            S.dma("sp", out=DE[kq:kq + 1, :], in_=tab_d[:, 127 - kq:127 - kq + 256], stream="DE", reads=[tab_b], writes=[DE_b])
        DEh = S.sb("DEh", [128, 256], BF16); DEl = S.sb("DEl", [128, 256], BF16); DEh_b = Buf("DEh")
        S.phase = "po"
        DEt, DEt_b = cst("DEt", [128, 256])
        S.phase = "g"
        S.op("dve", "tensor_copy", out=DEh[:], in_=DE[:], reads=[DE_b], writes=[DEh_b])
        S.op("dve", "tensor_copy", out=DEt[:], in_=DEh[:], reads=[DEh_b], writes=[DEt_b])
        S.op("dve", "tensor_tensor", out=DEt[:], in0=DE[:], in1=DEt[:], op=ALU.subtract, reads=[DE_b, DEt_b], writes=[DEt_b])
        S.op("dve", "tensor_copy", out=DEl[:], in_=DEt[:], reads=[DEt_b], writes=[DEh_b])
        S.op("dve", "tensor_scalar", out=subg[:], in0=subg[:], scalar1=1.0 - LAMBDA_INIT, scalar2=None, op0=ALU.mult, reads=[subg_b], writes=[subg_b])
        S.op("dve", "tensor_tensor", out=lamt[:, 0, :], in0=lamt[:, 0, :], in1=lamt[:, 1, :], op=ALU.mult, reads=[lamt_b], writes=[lamt_b])
        S.op("dve", "tensor_tensor", out=lamt[:, 2, :], in0=lamt[:, 2, :], in1=lamt[:, 3, :], op=ALU.mult, reads=[lamt_b], writes=[lamt_b])
        S.op("dve", "tensor_reduce", out=lamw[:, 0:1], in_=lamt[:, 0, :], axis=AX.X, op=ALU.add, reads=[lamt_b], writes=[lamw_b])
        S.op("dve", "tensor_reduce", out=lamw[:, 1:2], in_=lamt[:, 2, :], axis=AX.X, op=ALU.add, reads=[lamt_b], writes=[lamw_b])
        S.op("act", "activation", out=lamw[:, 2:4], in_=lamw[:, 0:2], func=AF.Exp, reads=[lamw_b], writes=[lamw_b])
        S.op("dve", "tensor_tensor", out=lamw[:, 4:5], in0=lamw[:, 3:4], in1=lamw[:, 2:3], op=ALU.subtract, reads=[lamw_b], writes=[lamw_b])
        S.op("dve", "tensor_scalar", out=lamw[:, 4:5], in0=lamw[:, 4:5], scalar1=-LAMBDA_INIT, scalar2=None, op0=ALU.add, reads=[lamw_b], writes=[lamw_b])

        kTb = S.sb("kTb", [128, 8192], BF16); kTb_b = [Buf("kTb%d" % i) for i in range(16)]
        Vaug = S.sb("Vaug", [128, 64, 130], BF16); Vaug_b = [Buf("Vaug%d" % i) for i in range(16)]
        vaug1_b = Buf("vaug_ones")
        S.op("pool", "memset", Vaug[:, :, 128:130], 1.0, writes=[vaug1_b] + Vaug_b)
        S.phase = "po"
        Qblk = S.sb("Qblk", [128, 2, 512], BF16); qTb_b = Buf("Qblk")
        S.op("pool", "memset", Qblk[:].rearrange("p a b -> p (a b)"), 0.0, writes=[qTb_b])
        PT = [S.sb("PT%d" % i, [128, 2, 256], BF16) for i in range(2)]; PT_b = [Buf("PT%d" % i) for i in range(2)]
        S.phase = "g"
        oaT, oaT_b = cst("oaT", [128, 512])
        S.phase = "po"
        ob_, ob_b = cst("o_blk", [128, 128]); osq, osq_b = DEt, DEt_b; fin, fin_b = cst("fin", [128, 8])
        S.phase = "g"
        pQK = [S.ps("pQK%d" % i, [128, 512], F32) for i in range(2)]; pQK_b = [Buf("pQK%d" % i) for i in range(2)]
        pAC = [S.ps("pAC%d" % i, [128, 512], F32) for i in range(2)]; pAC_b = [Buf("pAC%d" % i) for i in range(2)]
        acc_loc = {(0, 0): (0, 0), (0, 1): (0, 130), (1, 0): (0, 260), (1, 1): (1, 0)}
        jj = [0]

        def attn_group(g, gs):
            gi = g % 16
            S.op("pool", "tensor_copy", out=kTb[:, gi * 512:(gi + 1) * 512], in_=kn[:], reads=[kn_b], writes=[kTb_b[gi]])
            S.op("pool", "tensor_copy", out=Qblk[0:64, :, 0:256], in_=qn[0:64, :].rearrange("p (h q) -> p h q", h=2), reads=[qn_b], writes=[qTb_b])
            S.op("pool", "tensor_copy", out=Qblk[64:128, :, 256:512], in_=qn[64:128, :].rearrange("p (h q) -> p h q", h=2), reads=[qn_b], writes=[qTb_b])
            S.op("pool", "tensor_copy", out=Vaug[:, 4 * gi:4 * gi + 4, 0:128], in_=v_o[gs][:].rearrange("p (t d) -> p t d", d=128),
                 reads=[v_o_b[gs]], writes=[Vaug_b[gi]])
            for hf in range(2):
                i0 = 4 * gi + 2 * hf
                for j in range(i0 + 2):
                    lo = max(0, j - i0)
                    c0 = lo * 128
                    s = jj[0] % 2; jj[0] += 1
                    kb = kTb_b[j // 4]
                    nb = 0
                    biases = []
                    if j == i0 - 1:
                        biases = [(0, 128, 128, 256)]
                    elif j == i0:
                        biases = [(0, 256, 0, 256)]
                    elif j == i0 + 1:
                        biases = [(128, 256, 0, 128)]
                    mm(pQK[s][:, :], pQK_b[s], kTb[:, j * 128:(j + 1) * 128], kb, Qblk[:, hf, :], qTb_b, True, not biases, skip=True)
                    for c in range(2):
                        for (a0_, a1_, d0, d1) in biases:
                            mm(pQK[s][:, c * 256 + a0_:c * 256 + a1_], pQK_b[s], identb[:], identb_b, DEh[:, d0:d1], DEh_b, False, False, skip=True)
                            mm(pQK[s][:, c * 256 + a0_:c * 256 + a1_], pQK_b[s], identb[:], identb_b, DEl[:, d0:d1], DEh_b, False, True, skip=True)
                    S.op("act", "activation", out=PT[s][:, :, c0:256], in_=pQK[s][:].rearrange("p (c q) -> p c q", c=2)[:, :, c0:256],
                         func=AF.Exp, bias=b31c[:, 0:1], scale=ATT_SCALE, reads=[pQK_b[s], b31c_b], writes=[PT_b[s]])
                    for c in range(2):
                        for ll in range(lo, 2):
                            bk, col = acc_loc[(c, ll)]
                            mm(pAC[bk][:, col:col + 130], pAC_b[bk], PT[s][:, c, ll * 128:(ll + 1) * 128], PT_b[s],
                               Vaug[:, j, :], Vaug_b[j // 4], (j == 0 and (c, ll) in ((0, 0), (1, 1))), j == i0 + ll, skip=True)
                    yield
                for ll in range(2):
                    b0, c0_ = acc_loc[(0, ll)]; b1, c1_ = acc_loc[(1, ll)]
                    F = dict(reads=[fin_b], writes=[fin_b])
                    S.op("dve", "reciprocal", out=fin[:, 0:1], in_=pAC[b0][:, c0_ + 128:c0_ + 129], reads=[pAC_b[b0]], writes=[fin_b])
                    S.op("dve", "reciprocal", out=fin[:, 1:2], in_=pAC[b1][:, c1_ + 128:c1_ + 129], reads=[pAC_b[b1]], writes=[fin_b])
                    S.op("dve", "tensor_tensor", out=fin[:, 1:2], in0=fin[:, 1:2], in1=lamw[:, 4:5], op=ALU.mult, reads=[fin_b, lamw_b], writes=[fin_b])
                    S.op("dve", "tensor_scalar", out=ob_[:], in0=pAC[b0][:, c0_:c0_ + 128], scalar1=fin[:, 0:1], scalar2=None, op0=ALU.mult,
                         reads=[pAC_b[b0], fin_b], writes=[ob_b])
                    S.op("dve", "scalar_tensor_tensor", out=ob_[:], in0=pAC[b1][:, c1_:c1_ + 128], scalar=fin[:, 1:2], in1=ob_[:],
                         op0=ALU.mult, op1=ALU.add, reads=[pAC_b[b1], fin_b, ob_b], writes=[ob_b])
                    S.op("act", "activation", out=osq[:, 0:128], in_=ob_[:], func=AF.Square, accum_out=fin[:, 2:3], reads=[ob_b], writes=[osq_b, fin_b])
                    S.op("dve", "tensor_scalar", out=fin[:, 3:4], in0=fin[:, 2:3], scalar1=1.0 / 128, scalar2=RMS_EPS, op0=ALU.mult, op1=ALU.add, **F)
                    S.op("act", "activation", out=fin[:, 4:5], in_=fin[:, 3:4], func=AF.Sqrt, **F)
                    S.op("dve", "reciprocal", out=fin[:, 5:6], in_=fin[:, 4:5], **F)
                    S.op("dve", "tensor_scalar", out=ob_[:], in0=ob_[:], scalar1=fin[:, 5:6], scalar2=None, op0=ALU.mult,
                         reads=[ob_b, fin_b], writes=[ob_b])
                    p, pb = pnext()
                    S.op("pe", "transpose", out=p[:, 0:128], in_=ob_[:], identity=identf[:], reads=[ob_b, identf_b], writes=[pb])
                    q0 = (2 * hf + ll) * 128
                    S.op("dve", "tensor_scalar", out=oaT[:, q0:q0 + 128], in0=p[:, 0:128], scalar1=subg[:, 0:1], scalar2=None, op0=ALU.mult,
                         reads=[pb, subg_b], writes=[oaT_b])

        kvc_d = dr("kvc", [2560 * 128, 256])
        pt_d = dr("pt", [1, 2048], I32)
        wkv0_d = dr("wkv0", [128, 128, 64])
        sst_d = dr("sst", [6, 128, 128])
        rowv_d = dr("rowv_scratch", [5, 512, 128], kind="Internal")
        rowv_b = Buf("rowv_dram")
        SMP = {}

        def sample_setup():
            S.barrier()
            S.free_po()
            SMP["idx"] = S.sb("s_idx", [128, 512], I32), Buf("s_idx")
            SMP["ptb"] = S.sb("s_ptb", [128, 512], I32), Buf("s_ptb")
            SMP["idxf"] = cst("s_idxf", [128, 512])
            SMP["io"] = cst("s_io", [128, 1]); SMP["ioi"] = S.sb("s_ioi", [128, 1], I32), Buf("s_ioi")
            SMP["Qs"] = cst("s_Qs", [128, 128, 8])
            SMP["BN"] = cst("s_BN", [128, 32, 8]); SMP["BNt"] = cst("s_BNt", [128, 32, 8])
            SMP["BNh"] = S.sb("s_BNh", [128, 32, 8], BF16), Buf("s_BNh"); SMP["BNl"] = S.sb("s_BNl", [128, 32, 8], BF16), Buf("s_BNl")
            SMP["DEs"] = cst("s_DEs", [128, 8]); SMP["ones2"] = cst("s_ones2", [128, 2])
            SMP["PTs"] = [cst("s_PTs%d" % i, [128, 136]) for i in range(2)]
            SMP["OA"] = cst("s_OA", [4, 8, 260]); SMP["o1"] = cst("s_o1", [4, 8, 128]); SMP["o2"] = cst("s_o2", [4, 8, 128])
            SMP["fs"] = cst("s_fs", [4, 64])
            SMP["Bc"] = [cst("s_Bc%d" % i, [128, 8, 64]) for i in range(5)]
            SMP["Sst"] = cst("s_Sst", [128, 8, 64]); SMP["tS"] = cst("s_tS", [128, 8, 64]); SMP["sa"] = cst("s_sa", [128, 8])
            SMP["RV"] = cst("s_RV", [128, 512]); SMP["dec"] = cst("s_dec", [128, 512])
            SMP["sstt"] = cst("s_sstt", [128, 128]); SMP["zp"] = cst("s_zp", [128, 512])
            io, io_b = SMP["io"]; ioi, ioi_b = SMP["ioi"]
            S.op("pool", "iota", ioi[:], pattern=[[0, 1]], base=0, channel_multiplier=1, writes=[ioi_b])
            S.op("dve", "tensor_copy", out=io[:], in_=ioi[:], reads=[ioi_b], writes=[io_b])
            BN, BN_b = SMP["BN"]; BNt, BNt_b = SMP["BNt"]; BNh, BNh_b = SMP["BNh"]; BNl, BNl_b = SMP["BNl"]
            S.op("pool", "memset", BN[:].rearrange("p a b -> p (a b)"), -240000.0, writes=[BN_b])
            for sl in range(32):
                for c in range(2):
                    S.dma("sp", out=BN[sl * 4:(sl + 1) * 4, sl, c * 4:(c + 1) * 4], in_=DE[0:4, 0:4], stream="BN", reads=[DE_b], writes=[BN_b])
            fl = lambda ap: ap.rearrange("p a b -> p (a b)")
            S.op("dve", "tensor_copy", out=fl(BNh[:]), in_=fl(BN[:]), reads=[BN_b], writes=[BNh_b])
            S.op("dve", "tensor_copy", out=fl(BNt[:]), in_=fl(BNh[:]), reads=[BNh_b], writes=[BNt_b])
            S.op("dve", "tensor_tensor", out=fl(BNt[:]), in0=fl(BN[:]), in1=fl(BNt[:]), op=ALU.subtract, reads=[BN_b, BNt_b], writes=[BNt_b])
            S.op("dve", "tensor_copy", out=fl(BNl[:]), in_=fl(BNt[:]), reads=[BNt_b], writes=[BNl_b])
            DEs, DEs_b = SMP["DEs"]
            for c in range(2):
                S.op("dve", "tensor_copy", out=DEs[:, c * 4:(c + 1) * 4], in_=DE[:, 128:132], reads=[DE_b], writes=[DEs_b])
            ones2, ones2_b = SMP["ones2"]
            S.op("pool", "memset", ones2[:], 1.0, writes=[ones2_b])

        def sample_attn(gs):
            idx, idx_b = SMP["idx"]; ptb, ptb_b = SMP["ptb"]; idxf, idxf_b = SMP["idxf"]; io, io_b = SMP["io"]
            Qs, Qs_b = SMP["Qs"]; BN, BN_b = SMP["BN"]; DEs, DEs_b = SMP["DEs"]
            OA, OA_b = SMP["OA"]; o1, o1_b = SMP["o1"]; o2, o2_b = SMP["o2"]; fs, fs_b = SMP["fs"]
            allk = kTb_b + Vaug_b
            kp_b = [Buf("kp0"), Buf("kp1")]; vp_b = [Buf("vp0"), Buf("vp1")]; knb_b = kn_b; vn_b = v_o_b[gs]
            S.op("pool", "memset", Qs[:].rearrange("p a b -> p (a b)"), 0.0, writes=[Qs_b])
            S.op("pool", "tensor_copy", out=Qs[0:64, :, 0:4], in_=qn[0:64, :].rearrange("p (s t) -> p s t", t=4), reads=[qn_b], writes=[Qs_b])
            S.op("pool", "tensor_copy", out=Qs[64:128, :, 4:8], in_=qn[64:128, :].rearrange("p (s t) -> p s t", t=4), reads=[qn_b], writes=[Qs_b])
            KVv = [kTb[:].bitcast(F32).rearrange("p (g t) -> p g t", t=256),
                   Vaug[:].rearrange("p a b -> p (a b)")[:, 0:8192].bitcast(F32).rearrange("p (g t) -> p g t", t=256)]
            for q_ in range(2):
                S.op("pool", "memset", KVv[q_][:, 0, 0:1], 0.0, writes=[kp_b[q_]] + allk)
            ones2, ones2_b = SMP["ones2"]
            v4 = v_o[gs][:].rearrange("p (t d) -> p t d", d=128)
            for s in range(128):
                if s % 32 == 0:
                    c0 = s * 16
                    S.dma("sp", out=ptb[:], in_=pt_d[:, c0:c0 + 512].partition_broadcast(128), stream="s_ptb", writes=[ptb_b])
                    S.op("dve", "tensor_copy", out=idxf[:], in_=ptb[:], reads=[ptb_b], writes=[idxf_b])
                    S.op("dve", "tensor_scalar", out=idxf[:], in0=idxf[:], scalar1=128.0, scalar2=io[:, 0:1], op0=ALU.mult, op1=ALU.add,
                         reads=[idxf_b, io_b], writes=[idxf_b])
                    S.op("dve", "tensor_copy", out=idx[:], in_=idxf[:], reads=[idxf_b], writes=[idx_b])
                sl = s % 2
                ti = s // 32
                KVs = KVv[sl]
                for pg in range(16):
                    ic = (s % 32) * 16 + pg
                    S.dma("pool", out=KVs[:, pg, 0:256], in_=kvc_d, stream="kp%d" % sl, reads=[idx_b], writes=[kp_b[sl]], meth="indirect_dma_start",
                          out_offset=None, in_offset=bass.IndirectOffsetOnAxis(ap=idx[:, ic:ic + 1], axis=0))
                pss, pss_b = pQK[s % 2], pQK_b[s % 2]
                for pg in range(16):
                    mm(pss[:, pg * 8:(pg + 1) * 8], pss_b, KVs[:, pg, 0:128], kp_b[sl], Qs[:, s, :], Qs_b, True, pg != 15, skip=True)
                mm(pss[:, 120:128], pss_b, identf[:], identf_b, DEs[:, :], DEs_b, False, True, skip=True)
                mm(pss[:, 128:136], pss_b, kn[:, ti * 128:(ti + 1) * 128], knb_b, Qs[:, s, :], Qs_b, True, False, skip=True)
                mm(pss[:, 128:136], pss_b, identf[:], identf_b, BN[:, s % 32, :], BN_b, False, True, skip=True)
                PTs, PTs_b = SMP["PTs"][s % 2]
                S.op("act", "activation", out=PTs[:], in_=pss[:, 0:136], func=AF.Exp, bias=b31c[:, 0:1], scale=ATT_SCALE,
                     reads=[pss_b, b31c_b], writes=[PTs_b])
                pa, pa_b = pAC[s % 2], pAC_b[s % 2]
                for c in range(2):
                    for pg in range(17):
                        rhs, rb = (KVs[:, pg, 128:256], kp_b[sl]) if pg < 16 else (v4[:, ti, :], vn_b)
                        mm(pa[0:4, c * 130:c * 130 + 128], pa_b, PTs[:, pg * 8 + c * 4:pg * 8 + c * 4 + 4], PTs_b, rhs, rb,
                           (c == 0 and pg == 0), pg == 16, skip=True)
                        mm(pa[0:4, c * 130 + 128:c * 130 + 130], pa_b, PTs[:, pg * 8 + c * 4:pg * 8 + c * 4 + 4], PTs_b, ones2[:], ones2_b,
                           False, pg == 16, skip=True)
                k8 = s % 8
                S.op("act", "copy", out=OA[0:4, k8, :], in_=pa[0:4, 0:260], reads=[pa_b], writes=[OA_b])
                if k8 == 7:
                    b8 = s // 8
                    bc = lambda ap: ap.broadcast_to([4, 8, 128])
                    S.op("dve", "reciprocal", out=fs[:, 0:8], in_=OA[:, :, 128], reads=[OA_b], writes=[fs_b])
                    S.op("dve", "reciprocal", out=fs[:, 8:16], in_=OA[:, :, 258], reads=[OA_b], writes=[fs_b])
                    S.op("dve", "tensor_scalar", out=fs[:, 8:16], in0=fs[:, 8:16], scalar1=lamw[0:4, 4:5], scalar2=None, op0=ALU.mult,
                         reads=[fs_b, lamw_b], writes=[fs_b])
                    S.op("dve", "tensor_tensor", out=o1[:], in0=OA[:, :, 0:128], in1=bc(fs[:, 0:8].unsqueeze(2)), op=ALU.mult,
                         reads=[OA_b, fs_b], writes=[o1_b])
                    S.op("dve", "tensor_tensor", out=o2[:], in0=OA[:, :, 130:258], in1=bc(fs[:, 8:16].unsqueeze(2)), op=ALU.mult,
                         reads=[OA_b, fs_b], writes=[o2_b])
                    S.op("dve", "tensor_tensor", out=o1[:], in0=o1[:], in1=o2[:], op=ALU.add, reads=[o1_b, o2_b], writes=[o1_b])
                    S.op("dve", "tensor_tensor", out=o2[:], in0=o1[:], in1=o1[:], op=ALU.mult, reads=[o1_b], writes=[o2_b])
                    S.op("dve", "tensor_reduce", out=fs[:, 16:24], in_=o2[:], axis=AX.X, op=ALU.add, reads=[o2_b], writes=[fs_b])
                    S.op("dve", "tensor_scalar", out=fs[:, 24:32], in0=fs[:, 16:24], scalar1=1.0 / 128, scalar2=RMS_EPS, op0=ALU.mult, op1=ALU.add,
                         reads=[fs_b], writes=[fs_b])
                    S.op("act", "activation", out=fs[:, 32:40], in_=fs[:, 24:32], func=AF.Sqrt, reads=[fs_b], writes=[fs_b])
                    S.op("dve", "reciprocal", out=fs[:, 40:48], in_=fs[:, 32:40], reads=[fs_b], writes=[fs_b])
                    S.op("dve", "tensor_tensor", out=o1[:], in0=o1[:], in1=bc(fs[:, 40:48].unsqueeze(2)), op=ALU.mult,
                         reads=[o1_b, fs_b], writes=[o1_b])
                    p, pb = pnext()
                    for k in range(8):
                        S.op("pe", "transpose", out=p[:, k * 4:(k + 1) * 4], in_=o1[0:4, k, :], identity=identf[0:4, 0:4],
                             reads=[o1_b, identf_b], writes=[pb])
                    S.op("dve", "tensor_scalar", out=oaT[:, b8 * 32:(b8 + 1) * 32], in0=p[:, 0:32], scalar1=subg[:, 0:1], scalar2=None, op0=ALU.mult,
                         reads=[pb, subg_b], writes=[oaT_b])
                yield

        def sample_shift():
            sstt, sstt_b = SMP["sstt"]; zp, zp_b = SMP["zp"]
            tmpA, tmpA_b = WK["tmpA"]
            z4 = lambda ap: ap.rearrange("p (s t) -> p s t", t=4)
            for k, zi in enumerate([2, 3, 4, 7, 8, 9]):
                rows = 32 if zi == 9 else 128
                S.dma("sp", out=sstt[0:rows, :], in_=sst_d[k, 0:rows, :], stream="s_sstt", writes=[sstt_b])
                S.op("pool", "tensor_copy", out=z4(zp[0:rows, :])[:, :, 0], in_=sstt[0:rows, :], reads=[sstt_b], writes=[zp_b])
                S.op("pool", "tensor_copy", out=z4(zp[0:rows, :])[:, :, 1:4], in_=z4(zt[zi][0:rows, 1:513])[:, :, 0:3], reads=[zt_b[zi]], writes=[zp_b])
                S.op("dve", "tensor_tensor", out=tmpA[0:rows, :], in0=zp[0:rows, :], in1=zt[zi][0:rows, 1:513], op=ALU.subtract,
                     reads=[zp_b, zt_b[zi]], writes=[tmpA_b])
                S.op("dve", "scalar_tensor_tensor", out=zsh[k][0:rows, :], in0=tmpA[0:rows, :], scalar=rwc[0:rows, k:k + 1],
                     in1=zt[zi][0:rows, 1:513], op0=ALU.mult, op1=ALU.add, reads=[tmpA_b, zt_b[zi], rwc_b], writes=[zsh_b[k]])

        def sample_scan():
            RV, RV_b = SMP["RV"]; dec, dec_b = SMP["dec"]; Sst, Sst_b = SMP["Sst"]; tS, tS_b = SMP["tS"]; sa, sa_b = SMP["sa"]
            Bc = SMP["Bc"]
            ld, ld_b = WK["ld"]; kk, kk_b = WK["kk"]; kka, kka_b = WK["kka"]; kf, kf_b = WK["kf"]
            yT, yT_b = WK["yT"]
            S.op("act", "activation", out=dec[:], in_=ld[:], func=AF.Exp, reads=[ld_b], writes=[dec_b])
            vecs = [(kk, kk_b), (kka, kka_b), (dec, dec_b), (kf, kf_b), (zsh[0], zsh_b[0])]
            for vi, (vt, vb) in enumerate(vecs):
                p, pb = pnext()
                for ti in range(4):
                    S.op("pe", "transpose", out=p[:, ti * 128:(ti + 1) * 128], in_=vt[:, ti * 128:(ti + 1) * 128], identity=identf[:],
                         reads=[vb, identf_b], writes=[pb])
                S.op("act", "copy", out=RV[:], in_=p[:], reads=[pb], writes=[RV_b])
                S.dma("sp", out=rowv_d[vi].rearrange("(t p) d -> p t d", p=128), in_=RV[:].rearrange("p (t d) -> p t d", d=128),
                      stream="s_RV", reads=[RV_b], writes=[rowv_b])
            v4 = zsh[2][:].rearrange("p (s t) -> p s t", t=4)
            y4 = yT[:].rearrange("p (s t) -> p s t", t=4)
            for b8 in range(16):
                s0 = b8 * 8
                S.dma("sp", out=Sst[:], in_=wkv0_d[s0:s0 + 8].rearrange("s p j -> p s j"), stream="s_Sst", writes=[Sst_b])
                for t in range(4):
                    for vi in range(5):
                        bt, bb = Bc[vi]
                        for hh in range(2):
                            src = rowv_d[vi, s0 * 4 + t:(s0 + 7) * 4 + t + 1:4, hh * 64:(hh + 1) * 64].partition_broadcast(64)
                            S.dma("sp", out=bt[hh * 64:(hh + 1) * 64, :, :], in_=src, stream="s_Bc%d" % vi, reads=[rowv_b], writes=[bb])
                    (KKb, KKb_b), (KKAb, KKAb_b), (Db, Db_b), (Kb_, Kb_b), (Rb, Rb_b) = Bc
                    bc = lambda ap: ap.unsqueeze(2).broadcast_to([128, 8, 64])
                    S.op("dve", "tensor_tensor", out=tS[:], in0=Sst[:], in1=KKb[:], op=ALU.mult, reads=[Sst_b, KKb_b], writes=[tS_b])
                    S.op("dve", "tensor_reduce", out=sa[:], in_=tS[:], axis=AX.X, op=ALU.add, reads=[tS_b], writes=[sa_b])
                    S.op("dve", "tensor_tensor", out=Sst[:], in0=Sst[:], in1=Db[:], op=ALU.mult, reads=[Sst_b, Db_b], writes=[Sst_b])
                    S.op("pool", "tensor_tensor", out=tS[:], in0=KKAb[:], in1=bc(sa[:]), op=ALU.mult, reads=[KKAb_b, sa_b], writes=[tS_b])
                    S.op("dve", "tensor_tensor", out=Sst[:], in0=Sst[:], in1=tS[:], op=ALU.subtract, reads=[Sst_b, tS_b], writes=[Sst_b])
                    S.op("pool", "tensor_tensor", out=tS[:], in0=Kb_[:], in1=bc(v4[:, s0:s0 + 8, t]), op=ALU.mult, reads=[Kb_b, zsh_b[2]], writes=[tS_b])
                    S.op("dve", "tensor_tensor", out=Sst[:], in0=Sst[:], in1=tS[:], op=ALU.add, reads=[Sst_b, tS_b], writes=[Sst_b])
                    S.op("pool", "tensor_tensor", out=tS[:], in0=Sst[:], in1=Rb[:], op=ALU.mult, reads=[Sst_b, Rb_b], writes=[tS_b])
                    S.op("dve", "tensor_reduce", out=y4[:, s0:s0 + 8, t], in_=tS[:], axis=AX.X, op=ALU.add, reads=[tS_b], writes=[yT_b])
                    yield
                S.dma("sp", out=wkv_d[2 + s0:2 + s0 + 8].rearrange("s p j -> p s j"), in_=Sst[:], stream="s_Sout", reads=[Sst_b], is_output=True)

        mtiles = [(0, 128, 0), (128, 128, 1), (384, 128, 2), (512, 128, 3), (640, 128, 4), (768, 128, 5), (896, 128, 6),
                  (1024, 128, 7), (1152, 128, 8), (1280, 32, 9)]
        pcnt = 0
        if groups is None:
            groups = list(range(n_groups))
        for g in groups:
            if g == 32 and (do_attn or do_rwkv):
                sample_setup()
            gs = g % 2
            t0 = g * 512
            for ti in range(4):
                s = (g * 4 + ti) % 2
                r0 = t0 + ti * 128
                S.dma("sp", out=xts[s][:], in_=x_d[r0:r0 + 128, :], stream="xts%d" % s, writes=[xts_b[s]])
                rmsnorm_T(S, "attn", xts[s][:], xts_b[s], gat, identb, hT[gs], hT_b[gs], ti * 128, ptr, ptr_b, scr, cb=[identb_b, gat_b])
            for (c0, rows, zi) in mtiles:
                pp = pcnt % 3; pcnt += 1
                for c in range(8):
                    S.op("pe", "matmul", pP[pp][0:rows, :], lhsT=W[:, c, c0:c0 + rows], rhs=hT[gs][:, c, :],
                         start=(c == 0), stop=(c == 7), reads=[W_b, hT_b[gs]], writes=[pP_b[pp]])
                if zi in (2, 3, 4, 7, 8, 9):
                    if g in (0, 16, 32):
                        S.op("pool", "memset", zt[zi][0:rows, 0:1], 0.0, writes=[zt_b[zi]])
                    else:
                        S.op("pool", "tensor_copy", out=zt[zi][0:rows, 0:1], in_=zt[zi][0:rows, 512:513], reads=[zt_b[zi]], writes=[zt_b[zi]])
                S.op("act", "copy", out=zt[zi][0:rows, 1:513], in_=pP[pp][0:rows, :], reads=[pP_b[pp]], writes=[zt_b[zi]])
            for ti in range(4):
                for c in range(8):
                    S.op("pe", "matmul", pV[:, ti * 128:(ti + 1) * 128], lhsT=hT[gs][:, c, ti * 128:(ti + 1) * 128],
                         rhs=W[:, c, 256:384], start=(c == 0), stop=(c == 7), reads=[W_b, hT_b[gs]], writes=[pV_b])
            S.op("dve", "tensor_copy", out=v_o[gs][:], in_=pV[:], reads=[pV_b], writes=[v_o_b[gs]])
            S.dma("sp", out=vout_d[t0:t0 + 512, :].rearrange("(t p) d -> p t d", p=128),
                  in_=v_o[gs][:].rearrange("p (t d) -> p t d", d=128), stream="vo%d" % gs, reads=[v_o_b[gs]], is_output=True)
            for qi in range(2):
                S.op("act", "activation", out=sqf[:], in_=zt[qi][:, 1:513], func=AF.Square, reads=[zt_b[qi]], writes=[sqf_b])
                S.op("pe", "matmul", pS[:], lhsT=bones[:], rhs=sqf[:], start=True, stop=True,
                     reads=[bones_b, sqf_b], writes=[pS_b])
                S.op("dve", "tensor_scalar", out=rstd[:], in0=pS[:], scalar1=1.0 / 64, scalar2=QK_EPS, op0=ALU.mult, op1=ALU.add,
                     reads=[pS_b], writes=[rstd_b])
                S.op("act", "activation", out=rstd[:], in_=rstd[:], func=AF.Sqrt, reads=[rstd_b], writes=[rstd_b])
                S.op("dve", "reciprocal", out=rstd[:], in_=rstd[:], reads=[rstd_b], writes=[rstd_b])
                dst, dst_b = (qn, qn_b) if qi == 0 else (kn, kn_b)
                S.op("dve", "scalar_tensor_tensor", out=dst[:], in0=zt[qi][:, 1:513], scalar=gqk[:, qi:qi + 1], in1=rstd[:],
                     op0=ALU.mult, op1=ALU.mult, reads=[zt_b[qi], rstd_b, gqk_b], writes=[dst_b])
            for ti in range(4):
                S.op("pe", "transpose", out=pS[:, ti * 128:(ti + 1) * 128], in_=kn[:, ti * 128:(ti + 1) * 128], identity=identf[:],
                     reads=[kn_b, identf_b], writes=[pS_b])
            S.op("act", "copy", out=kT_o[gs][:], in_=pS[:], reads=[pS_b], writes=[kT_o_b[gs]])
            S.dma("sp", out=kout_d[t0:t0 + 512, :].rearrange("(t p) d -> p t d", p=128),
                  in_=kT_o[gs][:].rearrange("p (t d) -> p t d", d=128), stream="kTo%d" % gs, reads=[kT_o_b[gs]], is_output=True)
            ga = None
            if do_attn:
                if g < 32:
                    ga = attn_group(g, gs)
                else:
                    ga = sample_attn(gs)
            gr = rwkv_group(g, sample=(g == 32)) if do_rwkv else None
            na = (8 * (g % 16) + 6) if ga is not None else 0
            ratio = max(1, -(-na // 48))
            if g == 32:
                ratio = 2
            while ga is not None or gr is not None:
                if gr is not None:
                    try:
                        next(gr)
                    except StopIteration:
                        gr = None
                for _ in range(ratio):
                    if ga is not None:
                        try:
                            next(ga)
                        except StopIteration:
                            ga = None
            if do_rwkv:
                mo, mo_b = mTo[g % 2]
                if do_attn:
                    sga, sga_b = WK["sgb"]
                    S.op("act", "activation", out=sga[:], in_=zt[5][:, 1:513], func=AF.Sigmoid, reads=[zt_b[5]], writes=[sga_b])
                    S.op("dve", "tensor_tensor", out=sga[:], in0=sga[:], in1=oaT[:], op=ALU.mult, reads=[sga_b, oaT_b], writes=[sga_b])
                    S.op("dve", "tensor_tensor", out=mo[:], in0=mo[:], in1=sga[:], op=ALU.add, reads=[mo_b, sga_b], writes=[mo_b])
                S.dma("sp", out=mT_d[:, t0:t0 + 512], in_=mo[:], stream="mTo%d" % (g % 2), reads=[mo_b], is_output=True)
            zrows = [2, 3, 4, 7, 8, 9]
            if g in (15, 31):
                col = 0 if g == 15 else 1
                for k, zi in enumerate(zrows):
                    S.op("dve", "tensor_copy", out=zs[0:(32 if zi == 9 else 128), k, col:col + 1], in_=zt[zi][0:(32 if zi == 9 else 128), 512:513], reads=[zt_b[zi]], writes=[zs_b])
            if g == 32:
                for k, zi in enumerate(zrows):
                    S.op("dve", "tensor_copy", out=zs[0:(32 if zi == 9 else 128), k, 2:130], in_=zt[zi][0:(32 if zi == 9 else 128), 1:513].rearrange("p (s t) -> p s t", t=4)[:, :, 3],
                         reads=[zt_b[zi]], writes=[zs_b])
        S.dma("sp", out=zs_d.rearrange("k p n -> p k n"), in_=zs[:], stream="zs", reads=[zs_b], is_output=True)
        S.emit()
        if S.phase != "s":
            S.free_po()
        if plan is None:
            return S.plan
    return nc

OFF_K, OFF_V, OFF_RW, OFF_GATE = 1024, 2048, 3072, 6432
_NC_CACHE = {}


def _core_cols(c):
    cols = []
    cols += list(range(c * 128, (c + 1) * 128))
    cols += list(range(OFF_K + c * 128, OFF_K + (c + 1) * 128))
    cols += list(range(OFF_V + c * 128, OFF_V + (c + 1) * 128))
    for j in range(3):
        cols += list(range(OFF_RW + j * 1024 + c * 128, OFF_RW + j * 1024 + (c + 1) * 128))
    cols += list(range(OFF_GATE + c * 128, OFF_GATE + (c + 1) * 128))
    cols += list(range(OFF_GATE + 1024 + c * 128, OFF_GATE + 1024 + (c + 1) * 128))
    cols += list(range(OFF_RW + 3072, OFF_RW + 3360))
    return np.array(cols)


def _consts():
    f32 = np.float32
    bo = np.kron(np.eye(2), np.ones((64, 64))).astype(f32)
    su = np.triu(np.ones((64, 64)), 1); iu = np.triu(np.ones((64, 64)), 0)
    bd = lambda m: np.kron(np.eye(2), m).astype(f32)
    scanm = np.ones((128, 512), f32); scanm[:, ::64] = 0
    rel = np.arange(-127, 256)
    n = np.maximum(rel, 0); nf = np.maximum(n, 1).astype(f32)
    large = 16 + (np.log(nf / 16) / math.log(128 / 16) * 16).astype(np.int32); large = np.minimum(large, 31)
    bucket = np.where(n < 16, n, large); bucket = np.where(rel < 0, 32, bucket)
    oh = np.zeros((33, 383), f32); oh[bucket, np.arange(383)] = 1
    return dict(ident=np.eye(128, dtype=f32), blockones=bo, maskAR=np.concatenate([bd(su), bd(iu)], 1), maskSL=bd(su.T),
                scanm=scanm, bucket_onehot=oh)


def kernel(x_prompt, x_sample, p_prompt, p_sample, cache_k, cache_v, state_wkv, state_shift,
           page_table, rel_bias, attn_norm, w_in, q_norm, k_norm, lambda_q1, lambda_k1,
           lambda_q2, lambda_k2, subln_norm, rw_mu, rw_w0, rw_w2, rw_a0, rw_a2, rw_g2,
           rw_k_k, rw_k_a, rw_r_k, rw_gn_w, rw_gn_b, w_out, ffn_norm, w_grp, b_grp, w_exp,
           b_exp, w_gate, w_up, w_down, ple_norm, w_ple_gate, w_ple_proj):
    f32 = np.float32
    A = lambda a: np.ascontiguousarray(np.asarray(a, dtype=f32))
    xp = A(x_prompt).reshape(16384, 1024)
    xs = A(x_sample).reshape(512, 1024)
    x_all = np.concatenate([xp, xs], 0)
    w_in0 = A(w_in)[0]
    ident = np.eye(128, dtype=f32)
    gat = A(A(attn_norm)[0].reshape(8, 128).T)
    gqk = A(np.stack([np.tile(A(q_norm)[0], 2), np.tile(A(k_norm)[0], 2)], 1))
    if "A" not in _NC_CACHE:
        _NC_CACHE["A"] = build_A(do_attn=True, do_rwkv=True)
    C = _consts()
    mu = A(rw_mu)[0]; ck = A(cache_k)[0]; cv = A(cache_v)[0]; sw = A(state_wkv)[0]; ssh = A(state_shift)[0]
    lamv = A(np.stack([A(lambda_q1)[0], A(lambda_k1)[0], A(lambda_q2)[0], A(lambda_k2)[0]], 0))
    ptab = np.ascontiguousarray(np.asarray(page_table, dtype=np.int32).reshape(1, 2048))
    in_a = []
    for c in range(8):
        cs = slice(c * 128, (c + 1) * 128)
        rwc = np.zeros((128, 16), f32)
        rwc[:, 0] = mu[c * 128:(c + 1) * 128]; rwc[:, 1] = mu[1024 + c * 128:1024 + (c + 1) * 128]; rwc[:, 2] = mu[2048 + c * 128:2048 + (c + 1) * 128]
        rwc[:, 3] = mu[3072:3200]; rwc[:, 4] = mu[3200:3328]; rwc[:32, 5] = mu[3328:3360]
        for i, vv in enumerate([rw_w0, rw_a0, rw_k_k, rw_k_a, rw_r_k, rw_gn_w, rw_gn_b]):
            rwc[:, 6 + i] = A(vv)[0].reshape(-1)[cs]
        g2 = A(rw_g2)[0][:, cs]
        g2c = np.zeros((2, 128, 128), f32); g2c[0] = g2[:128]; g2c[1, :32] = g2[128:]
        rb = A(rel_bias)[:, c]
        sst = np.zeros((6, 128, 128), f32)
        sst[0] = ssh[:, c * 128:(c + 1) * 128].T; sst[1] = ssh[:, 1024 + c * 128:1024 + (c + 1) * 128].T
        sst[2] = ssh[:, 2048 + c * 128:2048 + (c + 1) * 128].T
        sst[3] = ssh[:, 3072:3200].T; sst[4] = ssh[:, 3200:3328].T; sst[5, :32] = ssh[:, 3328:3360].T
        in_a.append(dict(
            x=x_all, w=A(w_in0[:, _core_cols(c)]), gat=gat, gqk=gqk, rwc=rwc,
            w2a2=A(np.concatenate([A(rw_w2)[0][:, cs], A(rw_a2)[0][:, cs]], 0)), g2c=g2c,
            rbc=A(np.concatenate([rb, [0.0]])[:, None]), b31=A(rb[31:32][None]), lamv=lamv, subg=A(A(subln_norm)[0][:, None]),
            kvc=np.concatenate([ck[:, :, c, :].transpose(0, 2, 1), cv[:, :, c, :]], axis=2).reshape(2560 * 128, 256),
            pt=ptab, wkv0=np.ascontiguousarray(sw[:, 2 * c:2 * c + 2]).reshape(128, 128, 64), sst=sst, **C))
    ra = run_bass_kernel_spmd(_NC_CACHE["A"], in_a, core_ids=list(range(8))).results
    k_prompt = np.zeros((1, 2, 8192, 8, 128), f32); v_prompt = np.zeros((1, 2, 8192, 8, 128), f32)
    k_sample = np.zeros((1, 128, 4, 8, 128), f32); v_sample = np.zeros((1, 128, 4, 8, 128), f32)
    shift_prompt = np.zeros((1, 2, 3360), f32); shift_sample = np.zeros((1, 128, 3360), f32)
    wkv_prompt = np.zeros((1, 2, 16, 64, 64), f32); wkv_sample = np.zeros((1, 128, 16, 64, 64), f32)
    mT = np.zeros((1024, 16896), f32)
    for c in range(8):
        r = ra[c]
        k_prompt[0, :, :, c, :] = r["kout"][:16384].reshape(2, 8192, 128)
        v_prompt[0, :, :, c, :] = r["vout"][:16384].reshape(2, 8192, 128)
        k_sample[0, :, :, c, :] = r["kout"][16384:].reshape(128, 4, 128)
        v_sample[0, :, :, c, :] = r["vout"][16384:].reshape(128, 4, 128)
        zs = r["zs"]
        for j in range(3):
            shift_prompt[0, :, j * 1024 + c * 128:j * 1024 + (c + 1) * 128] = zs[j][:, 0:2].T
            shift_sample[0, :, j * 1024 + c * 128:j * 1024 + (c + 1) * 128] = zs[j][:, 2:130].T
        if c == 0:
            lr = np.concatenate([zs[3], zs[4], zs[5][:32]], 0)
            shift_prompt[0, :, 3072:] = lr[:, 0:2].T
            shift_sample[0, :, 3072:] = lr[:, 2:130].T
        if "mT" in r:
            mT[c * 128:(c + 1) * 128] = r["mT"]
        if "wkv" in r:
            wk = r["wkv"].reshape(130, 2, 64, 64)
            wkv_prompt[0, :, 2 * c:2 * c + 2] = wk[0:2]
            wkv_sample[0, :, 2 * c:2 * c + 2] = wk[2:130]
    pp = A(p_prompt)[0].reshape(16384, 256)
    psm = A(p_sample)[0].reshape(512, 256)
    if "B" not in _NC_CACHE:
        _NC_CACHE["B"] = build_B()
    wg_l = np.ascontiguousarray(A(w_gate)[0].reshape(32, 8, 128, 256).transpose(0, 2, 1, 3)).reshape(32, 128, 2048)
    wu_l = np.ascontiguousarray(A(w_up)[0].reshape(32, 8, 128, 256).transpose(0, 2, 1, 3)).reshape(32, 128, 2048)
    wd_l = np.ascontiguousarray(A(w_down)[0].reshape(32, 2, 128, 1024).transpose(0, 2, 1, 3)).reshape(32, 128, 2048)
    in_b = []
    for c in range(8):
        xb = np.zeros((TOKB, 1024), f32); pb = np.zeros((TOKB, 256), f32); mb = np.zeros((1024, TOKB), f32)
        xb[:2048] = xp[c * 2048:(c + 1) * 2048]; xb[2048:2112] = xs[c * 64:(c + 1) * 64]
        pb[:2048] = pp[c * 2048:(c + 1) * 2048]; pb[2048:2112] = psm[c * 64:(c + 1) * 64]
        mb[:, :2048] = mT[:, c * 2048:(c + 1) * 2048]; mb[:, 2048:2112] = mT[:, 16384 + c * 64:16384 + (c + 1) * 64]
        in_b.append(dict(mT=mb, x=xb, p=pb, w_out=A(w_out)[0], gff=A(A(ffn_norm)[0].reshape(8, 128).T),
                         gpl=A(A(ple_norm)[0].reshape(8, 128).T), wr=A(np.concatenate([A(w_grp)[0], A(w_exp)[0]], 1)),
                         br=A(np.concatenate([A(b_grp)[0], A(b_exp)[0]])[None]), w_gate=wg_l, w_up=wu_l,
                         w_down=wd_l, w_ple_gate=A(w_ple_gate)[0], w_ple_proj=A(w_ple_proj)[0], ident=ident))
    rb = run_bass_kernel_spmd(_NC_CACHE["B"], in_b, core_ids=list(range(8))).results
    y_prompt = np.zeros((16384, 1024), f32); y_sample = np.zeros((512, 1024), f32)
    for c in range(8):
        y = rb[c]["y"]
        y_prompt[c * 2048:(c + 1) * 2048] = y[:2048]
        y_sample[c * 64:(c + 1) * 64] = y[2048:2112]
    return (y_prompt.reshape(2, 8192, 1024), y_sample.reshape(128, 4, 1024), k_prompt, v_prompt, wkv_prompt, shift_prompt,
            k_sample, v_sample, wkv_sample, shift_sample)
```
